# Optimizing a Trainium2 kernel written in Bass

```python
import math
import jax, jax.numpy as jnp
from jax import lax
import numpy as np


D_MODEL = 1024
BATCH = 16
SEQ = 2048
DEPTH = 1

CHUNK = 64
H_A = 8
D_A = 64
H_I = 4
D_I = 64
TOPK_MAX = 256
SPARSE_Q_BLOCK = 32
H_B = 4
D_B = 64
DENSE_Q_BLOCK = 128
D_FF = ((8 * D_MODEL + 3 * 256 - 1) // (3 * 256)) * 256
RMS_EPS = 1e-6
MASK_VALUE = -1e30

W_QA = H_A * D_A
W_KA = H_A * D_A
W_VA = H_A * D_A
W_QB = H_B * 2 * D_B
W_KB = H_B * 2 * D_B
W_VB = H_B * 2 * D_B
W_QI = H_I * D_I
W_KI = D_I
W_WI = H_I
W_G = 2 * D_MODEL
_WIDTHS = [W_QA, W_KA, W_VA, W_QB, W_KB, W_VB, W_QI, W_KI, W_WI, W_G]
SPLITS = [int(v) for v in np.cumsum(_WIDTHS)[:-1]]
W_IN = int(sum(_WIDTHS))

kernel_name = 'hybrid_dsa_diffattn_gated_block'


def rms_norm(x, g):
    xf = x.astype(jnp.float32)
    y = xf * lax.rsqrt(jnp.mean(xf * xf, axis=-1, keepdims=True) + RMS_EPS)
    return (y * g.astype(jnp.float32)).astype(x.dtype)


def alibi_slopes(n):
    return jnp.asarray(2.0 ** (-8.0 * np.arange(1, n + 1) / n), dtype=jnp.float32)


def dsa_sparse_attention(q, k, v, q_idx, k_idx, w_idx):
    B, S = q.shape[0], q.shape[1]
    n_sel = min(TOPK_MAX, S // 4)
    nblk = S // SPARSE_Q_BLOCK
    slopes = alibi_slopes(H_A)
    key_chunk = jnp.arange(S) // CHUNK
    kif = k_idx.astype(jnp.float32)

    def to_blocks(a):
        return a.reshape((B, nblk, SPARSE_Q_BLOCK) + a.shape[2:]).swapaxes(0, 1)

    def block(args):
        i, qb, qib, wb = args
        t = i * SPARSE_Q_BLOCK + jnp.arange(SPARSE_Q_BLOCK)
        t_chunk = t // CHUNK
        admissible = key_chunk[None, :] <= t_chunk[:, None]
        logits = jnp.einsum('bqhd,bsd->bqhs', qib.astype(jnp.float32), kif)
        idx_score = jnp.einsum('bqh,bqhs->bqs', wb.astype(jnp.float32), jax.nn.relu(logits))
        idx_score = jnp.where(admissible[None], idx_score, -jnp.inf)
        _, sel = lax.top_k(idx_score, n_sel)
        valid = (sel // CHUNK) <= t_chunk[None, :, None]
        k_sel = jax.vmap(lambda kk, ii: kk[ii])(k, sel)
        v_sel = jax.vmap(lambda vv, ii: vv[ii])(v, sel)
        s = jnp.einsum('bqhd,bqkhd->bhqk', qb.astype(jnp.float32),
                       k_sel.astype(jnp.float32)) * (D_A ** -0.5)
        dist = jnp.abs(t[None, :, None] - sel).astype(jnp.float32)
        s = s - slopes[None, :, None, None] * dist[:, None]
        s = jnp.where(valid[:, None], s, MASK_VALUE)
        p = jax.nn.softmax(s, axis=-1)
        o = jnp.einsum('bhqk,bqkhd->bqhd', p, v_sel.astype(jnp.float32))
        return o.astype(q.dtype)

    out = lax.map(block, (jnp.arange(nblk), to_blocks(q), to_blocks(q_idx), to_blocks(w_idx)))
    return out.swapaxes(0, 1).reshape(B, S, H_A * D_A)


def differential_attention(q, k, v, lam, subln_g, lambda_init):
    B, S = q.shape[0], q.shape[1]
    nblk = S // DENSE_Q_BLOCK
    slopes = alibi_slopes(H_B)
    key_pos = jnp.arange(S)
    key_chunk = key_pos // CHUNK
    kf = k.astype(jnp.float32)
    vf = v.astype(jnp.float32)

    def block(args):
        i, qb = args
        t = i * DENSE_Q_BLOCK + jnp.arange(DENSE_Q_BLOCK)
        s = jnp.einsum('bqhmd,bshmd->bhmqs', qb.astype(jnp.float32), kf) * (D_B ** -0.5)
        dist = jnp.abs(t[:, None] - key_pos[None, :]).astype(jnp.float32)
        bias = -slopes[:, None, None, None] * dist[None, None]
        allowed = key_chunk[None, :] <= (t // CHUNK)[:, None]
        s = jnp.where(allowed, s + bias, MASK_VALUE)
        p = jax.nn.softmax(s, axis=-1)
        a = p[:, :, 0] - lam * p[:, :, 1]
        o = jnp.einsum('bhqs,bshe->bqhe', a, vf)
        o = rms_norm(o, subln_g) * (1.0 - lambda_init)
        return o.astype(q.dtype)

    qblk = q.reshape((B, nblk, DENSE_Q_BLOCK) + q.shape[2:]).swapaxes(0, 1)
    out = lax.map(block, (jnp.arange(nblk), qblk))
    return out.swapaxes(0, 1).reshape(B, S, H_B * 2 * D_B)


def setup_inputs(seed: int = 0) -> dict:
    key = jax.random.key(seed)
    ks = jax.random.split(key, 24)
    f32 = jnp.float32

    def w(k, shape, fan_in, gain=1.0):
        return jax.random.normal(k, shape, f32) * (gain * fan_in ** -0.5)

    def g(k, n):
        return 1.0 + 0.02 * jax.random.normal(k, (DEPTH, n), f32)

    return {
        'x': jax.random.normal(ks[0], (BATCH, SEQ, D_MODEL), f32),
        'c': jax.random.normal(ks[1], (BATCH, D_MODEL), f32),
        'w_ada': w(ks[2], (DEPTH, D_MODEL, 6 * D_MODEL), D_MODEL, 0.5),
        'b_ada': 0.02 * jax.random.normal(ks[3], (DEPTH, 6 * D_MODEL), f32),
        'norm1_g': g(ks[4], D_MODEL),
        'w_in': w(ks[5], (DEPTH, D_MODEL, W_IN), D_MODEL),
        'qn_a': g(ks[6], D_A),
        'kn_a': g(ks[7], D_A),
        'qn_b': g(ks[8], D_B),
        'kn_b': g(ks[9], D_B),
        'lam_q1': 0.1 * jax.random.normal(ks[10], (DEPTH, D_B), f32),
        'lam_k1': 0.1 * jax.random.normal(ks[11], (DEPTH, D_B), f32),
        'lam_q2': 0.1 * jax.random.normal(ks[12], (DEPTH, D_B), f32),
        'lam_k2': 0.1 * jax.random.normal(ks[13], (DEPTH, D_B), f32),
        'subln_g': g(ks[14], 2 * D_B),
        'w_up_a': w(ks[15], (DEPTH, H_A * D_A, D_MODEL), H_A * D_A),
        'w_up_b': w(ks[16], (DEPTH, H_B * 2 * D_B, D_MODEL), H_B * 2 * D_B),
        'w_o': w(ks[17], (DEPTH, D_MODEL, D_MODEL), D_MODEL),
        'norm2_g': g(ks[18], D_MODEL),
        'w_ff1': w(ks[19], (DEPTH, D_MODEL, D_FF), D_MODEL),
        'w_ff3': w(ks[20], (DEPTH, D_MODEL, D_FF), D_MODEL),
        'w_ff2': w(ks[21], (DEPTH, D_FF, D_MODEL), D_FF),
    }


def reference(x, c, w_ada, b_ada, norm1_g, w_in, qn_a, kn_a, qn_b, kn_b,
              lam_q1, lam_k1, lam_q2, lam_k2, subln_g, w_up_a, w_up_b, w_o,
              norm2_g, w_ff1, w_ff3, w_ff2):
    B, S = x.shape[0], x.shape[1]
    cs = jax.nn.silu(c)
    for l in range(DEPTH):
        lambda_init = 0.8 - 0.6 * math.exp(-0.3 * l)
        mod = cs @ w_ada[l] + b_ada[l]
        shift1, scale1, gate1, shift2, scale2, gate2 = jnp.split(mod, 6, axis=-1)

        h = rms_norm(x, norm1_g[l]) * (1.0 + scale1[:, None]) + shift1[:, None]
        proj = h @ w_in[l]
        qa, ka, va, qb, kb, vb, qi, ki, wi, gates = jnp.split(proj, SPLITS, axis=-1)

        qa = rms_norm(qa.reshape(B, S, H_A, D_A), qn_a[l])
        ka = rms_norm(ka.reshape(B, S, H_A, D_A), kn_a[l])
        va = va.reshape(B, S, H_A, D_A)
        y_a = dsa_sparse_attention(qa, ka, va, qi.reshape(B, S, H_I, D_I), ki, wi)

        qb = rms_norm(qb.reshape(B, S, H_B, 2, D_B), qn_b[l])
        kb = rms_norm(kb.reshape(B, S, H_B, 2, D_B), kn_b[l])
        vb = vb.reshape(B, S, H_B, 2 * D_B)
        lam = (jnp.exp(jnp.sum(lam_q1[l].astype(jnp.float32) * lam_k1[l].astype(jnp.float32)))
               - jnp.exp(jnp.sum(lam_q2[l].astype(jnp.float32) * lam_k2[l].astype(jnp.float32)))
               + lambda_init)
        y_b = differential_attention(qb, kb, vb, lam, subln_g[l], lambda_init)

        g_a, g_b = jnp.split(jax.nn.sigmoid(gates), 2, axis=-1)
        merged = g_a * (y_a @ w_up_a[l]) + g_b * (y_b @ w_up_b[l])
        x = x + gate1[:, None] * (merged @ w_o[l])

        h2 = rms_norm(x, norm2_g[l]) * (1.0 + scale2[:, None]) + shift2[:, None]
        ff = (jax.nn.silu(h2 @ w_ff1[l]) * (h2 @ w_ff3[l])) @ w_ff2[l]
        x = x + gate2[:, None] * ff
    return x
```

```python
import numpy as np
import ml_dtypes
import concourse.bass as bass
import concourse.mybir as mybir
from concourse.bass_utils import run_bass_kernel_spmd

F32 = mybir.dt.float32
BF16 = mybir.dt.bfloat16
ALU = mybir.AluOpType
AF = mybir.ActivationFunctionType
AX = mybir.AxisListType

S = 2048
D = 1024
DFF = 2816
NB = 2
NCORES = 8
W_IN = 5444
C_QA, C_KA, C_VA, C_QB, C_KB, C_VB, C_QI, C_KI, C_WI, C_G = 0, 512, 1024, 1536, 2048, 2560, 3072, 3328, 3392, 3396
NEG = -30000.0
TOPK = 256
NBIS = 26
EPS = 1e-6
SLOPES = [2.0 ** (-(h + 1)) for h in range(8)] + [2.0 ** (-2 * (h + 1)) for h in range(4)]


class Buf:
    __slots__ = ("name", "w", "r")

    def __init__(self, name):
        self.name = name
        self.w = {}
        self.r = {}


class Op:
    __slots__ = ("eng", "fn", "deps", "need", "cnt", "dma", "sem", "semval", "prevval")


class Sched:
    ENGS = ("pe", "act", "dve", "pool", "sp")

    def __init__(self):
        self.q = {e: [] for e in self.ENGS}
        self.bufs = {}
        self.dmas = []
        self.all_dmas = []

    def B(self, name):
        b = self.bufs.get(name)
        if b is None:
            b = self.bufs[name] = Buf(name)
        return b

    def _dep(self, o, t, hazard, strict=False):
        if t is o:
            return
        if not o.dma and not t.dma and t.eng == o.eng:
            if o.eng == "pe" or (hazard != "raw" and not strict):
                return
        o.deps.append(t)

    def op(self, eng, fn, reads=(), writes=(), dma=False):
        o = Op()
        o.eng, o.fn, o.dma, o.deps, o.need = eng, fn, dma, [], False
        o.cnt = 0
        for b in reads:
            for w in b.w.values():
                self._dep(o, w, "raw")
        for b in writes:
            st = b.name.startswith("junk")
            for w in b.w.values():
                self._dep(o, w, "waw", st)
            for r in b.r.values():
                self._dep(o, r, "war", st)
        for b in reads:
            b.r[id(o) if dma else eng] = o
        for b in writes:
            b.w = {(id(o) if dma else eng): o}
            b.r = {}
        self.q[eng].append(o)
        if dma:
            self.dmas.append(o)
            self.all_dmas.append(o)
        return o

    def barrier(self):
        lasts = []
        for e in self.ENGS:
            for o in reversed(self.q[e]):
                if o.fn is not None and not o.dma:
                    lasts.append(o)
                    break
        for e in self.ENGS:
            o = Op()
            o.eng, o.fn, o.dma, o.need, o.cnt = e, None, False, False, 0
            o.deps = [t for t in lasts if t.eng != e] + list(self.dmas)
            self.q[e].append(o)
        self.dmas = []
        for b in self.bufs.values():
            b.w = {}
            b.r = {}

    def prepare(self, sems, dma_sems):
        for e in self.ENGS:
            for o in self.q[e]:
                for t in o.deps:
                    t.need = True
        for e in self.ENGS:
            cnt = 0
            pool = dma_sems.get(e, [])
            uses = [0] * len(pool)
            rr = 0
            for o in self.q[e]:
                if o.dma:
                    i = rr % len(pool)
                    rr += 1
                    o.sem = pool[i]
                    o.prevval = 16 * uses[i]
                    uses[i] += 1
                    o.semval = 16 * uses[i]
                elif o.need:
                    cnt += 1
                    o.cnt = cnt
                    o.sem = sems[e]
                    o.semval = cnt

    def emit_engine(self, e, eng):
        waited = {}
        nw = 0
        ni = 0
        for o in self.q[e]:
            req = {}
            for t in o.deps:
                key = id(t.sem)
                if key not in req or req[key][1] < t.semval:
                    req[key] = (t.sem, t.semval)
            if o.dma and o.prevval > 0:
                key = id(o.sem)
                if key not in req or req[key][1] < o.prevval:
                    req[key] = (o.sem, o.prevval)
            for key, (sem, val) in req.items():
                if waited.get(key, 0) < val:
                    eng.wait_ge(sem, val)
                    waited[key] = val
                    nw += 1
            if o.fn is None:
                continue
            inst = o.fn(eng)
            ni += 1
            if o.dma:
                inst.then_inc(o.sem, 16)
            elif o.need:
                inst.then_inc(o.sem, 1)
        return (ni, nw)


def _consts():
    bf = ml_dtypes.bfloat16
    ident = np.eye(128, dtype=np.float32).astype(bf)
    bones = np.zeros((128, 128), np.float32)
    bones[:64, :64] = 1.0 / 64
    bones[64:, 64:] = 1.0 / 64
    omean = np.full((128, 128), 1.0 / 128, np.float32)
    ones = np.ones((128, 128), np.float32)
    tl = np.arange(512)
    qaug = np.zeros((128, 512), np.float32)
    qaug[4], qaug[5], qaug[6] = tl // 64, tl % 64, 1.0
    sl = np.arange(128)
    kaug = np.zeros((128, 12 * 128), np.float32)
    corr = np.zeros((128, 12 * 128), np.float32)
    for h, sp in enumerate(SLOPES):
        kaug[0:4, h * 128:(h + 1) * 128] = -sp
        kaug[4, h * 128:(h + 1) * 128] = -64.0 * sp
        kaug[5, h * 128:(h + 1) * 128] = -sp
        kaug[6, h * 128:(h + 1) * 128] = sp * sl
        ss, tt = np.meshgrid(sl, sl, indexing="ij")
        c = np.where((ss // 64) > (tt // 64), NEG,
                     np.where(ss > tt, -2.0 * sp * (ss - tt), 0.0))
        corr[:, h * 128:(h + 1) * 128] = c
    negblk = np.zeros((128, 128), np.float32)
    negblk[:64, 64:] = -1e30
    bk = np.zeros((128, 68), np.float32)
    for hd in range(4):
        for dl in range(-15, 2):
            bk[:, hd * 17 + dl + 15] = SLOPES[8 + hd] * (np.arange(128) + 128.0 * dl - 255.0)
    pp, cc = np.meshgrid(np.arange(128), np.arange(2048), indexing="ij")
    ndist = (-np.abs(pp + 1920 - cc)).astype(np.float32)
    tief = np.ascontiguousarray(np.broadcast_to((1.0 - (np.arange(2048) + 1.0) * 2.0 ** -12).astype(np.float32)[None, :], (128, 2048)))
    return dict(ident=ident, bones=bones.astype(bf), omean=omean.astype(bf), ones=ones.astype(bf),
                qaug=qaug.astype(bf), kaug=kaug.astype(bf), corr=corr.astype(bf), negblk=negblk, tief=tief, ndist=ndist.astype(bf), bk=bk)


CONST_SPECS = dict(ident=([128, 128], BF16), bones=([128, 128], BF16), omean=([128, 128], BF16),
                   ones=([128, 128], BF16), qaug=([128, 512], BF16), kaug=([128, 1536], BF16), ndist=([128, 2048], BF16),
                   corr=([128, 1536], BF16), negblk=([128, 128], F32),
                   tief=([128, 2048], F32), bk=([128, 68], F32))

IN_SPECS = dict(
    x=([NB, S, D], F32), c_fm=([128, 16], F32), w_ada=([D, 6 * D], F32), b_ada_fm=([128, 48], F32),
    bg=([128, 2048], F32), n1g=([128, 8], F32), n2g=([128, 8], F32), w_in=([D, W_IN], F32),
    qkg=([128, 4], F32), subg=([128, 1], F32), lamv=([1, 256], F32),
    w_up_a=([512, D], F32), w_up_b=([512, D], F32), w_o=([D, D], F32),
    w_ff1=([D, DFF], F32), w_ff3=([D, DFF], F32), w_ff2=([DFF, D], F32))


def build(nb=NB, dbg=None, stop_after=None):
    nc = bass.Bass("TRN2", target_bir_lowering=False)
    dr = {}
    for k, (shp, dt) in IN_SPECS.items():
        dr[k] = nc.dram_tensor(k, list(shp), dt, kind="ExternalInput").ap()
    for k, (shp, dt) in CONST_SPECS.items():
        dr[k] = nc.dram_tensor(k, list(shp), dt, kind="ExternalInput").ap()
    out_d = nc.dram_tensor("out", [NB, S, D], F32, kind="ExternalOutput").ap()
    dbg = dbg or {}
    dbg_d = {k: nc.dram_tensor("dbg_" + k, list(v[1]), v[2], kind="ExternalOutput").ap()
             for k, v in dbg.items()}

    sc = Sched()
    B = sc.B
    KB = 1024
    base = [16512]

    def region(nbytes):
        at = base[0]
        base[0] += nbytes
        return at

    def at(name, shape, dt, off):
        return nc.alloc_sbuf_tensor_at(name, list(shape), dt, offset=off)

    def salloc(name, shape, dt):
        nbytes = int(np.prod(shape[1:])) * (4 if dt == F32 else 2)
        nbytes = (nbytes + 63) // 64 * 64
        return at(name, shape, dt, region(nbytes))

    A0 = region(32 * KB)
    D0 = region(32 * KB)
    B0 = region(32 * KB)
    C0 = region(16 * KB)
    E0 = region(44 * KB)
    hT = at("hT", [128, 8, S], BF16, A0)
    x1 = at("x1", [128, 16, D], F32, A0)
    yT = at("yT", [128, 8, S], BF16, D0)
    MB = at("MB", [128, 2, S], BF16, D0 + 16 * KB)
    TIEF = at("TIEF", [128, S], F32, D0 + 24 * KB)
    QK = at("QK", [128, 8, S], BF16, B0)
    mrgT = at("mrgT", [128, 8, S], BF16, B0)
    h2T = at("h2T", [128, 8, S], BF16, B0)
    V = at("V", [128, 16, 512], BF16, C0)
    Wo = at("Wo", [128, 8, D], BF16, C0)
    IDX = at("IDX", [128, 2, S], F32, E0)
    MBT = at("MBT", [128, 16, 512], BF16, E0 + 16 * KB)
    QI = at("QI", [128, 2, S], BF16, E0 + 32 * KB)
    KI = at("KI", [128, S], BF16, E0 + 40 * KB)
    XT = at("XT", [128, 2, D], F32, E0)
    XN = at("XN", [128, 2, D], BF16, E0 + 8 * KB)
    JUNKE = at("JUNKE", [128, D], BF16, E0 + 12 * KB)
    CSR = at("CSR", [128, 8, 128], BF16, E0 + 16 * KB)
    GTf = at("GTf", [128, 2, S], BF16, E0)
    uT = at("uT", [128, 22, 1024], BF16, C0)
    W2 = at("W2", [128, 22, 256], BF16, C0 + 44 * KB)
    NSLOT = 2
    WB = salloc("WB", [128, NSLOT, 8, 256], BF16)
    JUNK8 = salloc("JUNK8", [128, S], mybir.dt.uint8)
    JUNKA = nc.alloc_sbuf_tensor_at("JUNKA", [128, D], BF16, offset=base[0] - 2048)
    BISU = salloc("BISU", [128, 2], mybir.dt.uint32)
    JUNK2 = salloc("JUNK2", [128, S], mybir.dt.uint8)
    PT0 = base[0]
    PT = salloc("PT", [128, 3, 512], BF16)

    def PT_at(name, shape, dt, off):
        return at(name, shape, dt, PT0 + off)

    SQ = salloc("SQ", [128, 2, 512], BF16)
    RS = salloc("RS", [128, 2, 512], F32)
    T1 = salloc("T1", [128, 2, 512], F32)
    GBC = salloc("GBC", [128, D], F32)
    ident = salloc("ident", [128, 128], BF16)
    bones = salloc("bones", [128, 128], BF16)
    omean = salloc("omean", [128, 128], BF16)
    ones = salloc("ones", [128, 128], BF16)
    qaug = salloc("qaug", [128, 512], BF16)
    qaugd = salloc("qaugd", [128, 512], BF16)
    kaug = salloc("kaug", [128, 1536], BF16)
    QZ = salloc("QZ", [128, 2, 512], BF16)
    ndist = salloc("ndist", [128, S], BF16)
    DMIN = salloc("DMIN", [128, 4], F32)
    BK = salloc("BK", [128, 68], F32)
    DMINX = salloc("DMINX", [128, 4, 4], BF16)
    corr = salloc("corr", [128, 1536], BF16)
    negblk = salloc("negblk", [128, 128], F32)
    cfm = salloc("cfm", [128, 16], F32)
    csT = salloc("csT", [128, 16], BF16)
    bfm = salloc("bfm", [128, 48], F32)
    modfm = salloc("modfm", [128, 96], F32)
    n1g = salloc("n1g", [128, 8], F32)
    n2g = salloc("n2g", [128, 8], F32)
    A1 = salloc("A1", [128, 16], F32)
    A2 = salloc("A2", [128, 16], F32)
    qkg = salloc("qkg", [128, 4], F32)
    subg = salloc("subg", [128, 1], F32)
    lamv = PT_at("lamv", [1, 256], F32, 0)
    lsc = PT_at("lsc", [1, 16], F32, 1024)
    onesf = PT_at("onesf", [1, 128], F32, 1152)
    nlam = salloc("nlam", [128, 1], F32)
    WI = salloc("WI", [128, 64], F32)
    ST = salloc("ST", [128, 16], F32)
    BIS = salloc("BIS", [128, 16], F32)
    EPSC = salloc("EPSC", [128, 1], F32)
    assert base[0] <= 229344, base[0]
    print("sbuf free bytes/partition:", 229344 - base[0])

    PS = [nc.alloc_psum_tensor("ps%d" % i, [128, 512], F32) for i in range(8)]
    PSB = [p.bitcast(BF16) for p in PS]
    psb = [B("ps%d" % i) for i in range(8)]
    CONST = B("const")

    def dma(q, out, in_, reads=(), writes=()):
        return sc.op(q, lambda e: e.dma_start(out=out, in_=in_), reads, writes, dma=True)

    def mm(out, lhsT, rhs, start, stop, reads, writes):
        return sc.op("pe", lambda e: e.matmul(out, lhsT, rhs, start=start, stop=stop), reads, writes)

    def tr(out, in_, reads, writes):
        return sc.op("pe", lambda e: e.transpose(out, in_, ident[:, :]), list(reads) + [CONST], writes)

    def act(out, in_, func, reads, writes, bias=None, scale=None, accum_out=None):
        kw = {}
        if bias is not None:
            kw["bias"] = bias
        if scale is not None:
            kw["scale"] = scale
        if accum_out is not None:
            kw["accum_out"] = accum_out
        return sc.op("act", lambda e: e.activation(out, in_, func, **kw), reads, writes)

    def ts(eng, out, in0, s1, s2, op0, op1, reads, writes, accum_out=None):
        kw = {}
        if accum_out is not None:
            kw["accum_out"] = accum_out
        if op1 is None:
            return sc.op(eng, lambda e: e.tensor_scalar(out, in0, s1, None, op0, **kw), reads, writes)
        return sc.op(eng, lambda e: e.tensor_scalar(out, in0, s1, s2, op0, op1, **kw), reads, writes)

    def stt(out, in0, scalar, in1, op0, op1, reads, writes):
        return sc.op("dve", lambda e: e.scalar_tensor_tensor(out, in0, scalar, in1, op0, op1), reads, writes)

    def tt(eng, out, in0, in1, op, reads, writes):
        return sc.op(eng, lambda e: e.tensor_tensor(out, in0, in1, op), reads, writes)

    def cp(eng, out, in_, reads, writes):
        if eng == "act":
            return sc.op("act", lambda e: e.copy(out, in_), reads, writes)
        return sc.op(eng, lambda e: e.tensor_copy(out, in_), reads, writes)

    def red(out, in_, op, reads, writes):
        return sc.op("dve", lambda e: e.tensor_reduce(out, in_, AX.X, op), reads, writes)

    def recip(out, in_, reads, writes):
        return sc.op("dve", lambda e: e.reciprocal(out, in_), reads, writes)

    def memset(ap, val, writes):
        return sc.op("dve", lambda e: e.memset(ap, val), (), writes)

    wslot = [0]
    WXA = [at("WXA%d" % i, [128, 8, 256], BF16, E0 + 8 * KB + i * 4 * KB) for i in range(4)]
    WXF = [at("WXF%d" % i, [128, 8, 256], BF16, C0 + 44 * KB + i * 4 * KB) for i in range(4)]
    SLOTS = {0: WB[:, 0, :, :], 1: WB[:, 1, :, :]}
    for i in range(4):
        SLOTS[2 + i] = WXA[i][:, :, :]
        SLOTS[6 + i] = WXF[i][:, :, :]
    active = [[0, 1]]

    def set_slots(lst):
        active[0] = list(lst)

    def WS(s):
        return SLOTS[s]

    def wbuf(s):
        return B("WB%d" % s)

    def next_slot():
        s = active[0][wslot[0] % len(active[0])]
        wslot[0] += 1
        return s

    def w3(name, c0, ncols, k0=0, nk=8):
        return dr[name].rearrange("(k p) c -> p k c", p=128)[:, k0:k0 + nk, c0:c0 + ncols]

    def load_w(name, c0, ncols, k0=0, nk=8, slot=None, kdst=0, cdst=0):
        s = next_slot() if slot is None else slot
        dma("pool", WS(s)[:, kdst:kdst + nk, cdst:cdst + ncols], w3(name, c0, ncols, k0, nk), writes=[wbuf(s)])
        return s

    for nm, t in [("ident", ident), ("bones", bones), ("omean", omean), ("ones", ones), ("qaug", qaug),
                  ("kaug", kaug), ("corr", corr), ("negblk", negblk), ("c_fm", cfm), ("ndist", ndist), ("qaug", qaugd), ("bk", BK),
                  ("b_ada_fm", bfm), ("n1g", n1g), ("n2g", n2g), ("qkg", qkg), ("subg", subg),
                  ("lamv", lamv)]:
        dma("sp", t[:, :], dr[nm], writes=[CONST])
    memset(EPSC[:, :], EPS, [CONST])
    memset(onesf[:, :], 1.0, [CONST])
    memset(lsc[:, :], 0.0, [CONST])
    sc.barrier()

    if stop_after == "consts":
        nb = 0
    act(csT[:, :], cfm[:, :], AF.Silu, [], [B("csT")])
    ts("dve", qkg[:, 0:1], qkg[:, 0:1], 0.125, None, ALU.mult, None, [], [B("qkg")])
    ts("dve", qkg[:, 2:3], qkg[:, 2:3], 0.125, None, ALU.mult, None, [B("qkg")], [B("qkg")])
    ts("dve", subg[:, :], subg[:, :], 0.8, None, ALU.mult, None, [], [B("subg")])
    tt("dve", lamv[0:1, 0:64], lamv[0:1, 0:64], lamv[0:1, 64:128], ALU.mult, [], [B("lamv")])
    tt("dve", lamv[0:1, 128:192], lamv[0:1, 128:192], lamv[0:1, 192:256], ALU.mult, [B("lamv")], [B("lamv")])
    red(lsc[0:1, 0:1], lamv[0:1, 0:64], ALU.add, [B("lamv")], [B("lsc")])
    red(lsc[0:1, 1:2], lamv[0:1, 128:192], ALU.add, [B("lamv")], [B("lsc")])
    act(lsc[0:1, 2:4], lsc[0:1, 0:2], AF.Exp, [B("lsc")], [B("lsc2")])
    tt("dve", lsc[0:1, 4:5], lsc[0:1, 3:4], lsc[0:1, 2:3], ALU.subtract, [B("lsc2")], [B("lsc3")])
    ts("dve", lsc[0:1, 5:6], lsc[0:1, 4:5], -0.2, None, ALU.add, None, [B("lsc3")], [B("lsc4")])
    mm(PS[7][:, 0:2], onesf[0:1, :], lsc[0:1, 5:7], True, True, [B("lsc4")], [psb[7]])
    cp("dve", nlam[:, :], PS[7][:, 0:1], [psb[7]], [B("nlam")])

    MP = PS[6]
    for g in [0, 1, 2, 3, 6, 7, 8, 9]:
        for hf in range(2):
            s = load_w("w_ada", g * 512 + hf * 256, 256)
            for jj in range(2):
                j = g * 4 + hf * 2 + jj
                for k in range(8):
                    mm(MP[:, j:j + 49:48], WS(s)[:, k, jj * 128:(jj + 1) * 128], csT[:, 2 * k:2 * k + 2],
                       k == 0, k == 7, [wbuf(s), B("csT")], [psb[6]])
    for b in range(2):
        for (c0_, c1_) in [(0, 16), (24, 40)]:
            tt("dve", modfm[:, b * 48 + c0_:b * 48 + c1_], MP[:, b * 48 + c0_:b * 48 + c1_], bfm[:, c0_:c1_], ALU.add,
               [psb[6]], [B("modfm")])
        stt(A1[:, b * 8:(b + 1) * 8], modfm[:, b * 48 + 8:b * 48 + 16], 1.0, n1g[:, 0:8], ALU.add, ALU.mult,
            [B("modfm")], [B("A1")])
        stt(A2[:, b * 8:(b + 1) * 8], modfm[:, b * 48 + 32:b * 48 + 40], 1.0, n2g[:, 0:8], ALU.add, ALU.mult,
            [B("modfm")], [B("A2")])
    sc.barrier()

    def gate_bc(b, gi):
        g0 = [2048, 5120][gi]
        dma("sp", GBC[:, :], dr["bg"][:, gi * 1024:(gi + 1) * 1024], writes=[B("GBC")])
        for k in range(8):
            ts("dve", CSR[:, k, :], ones[:, :], csT[:, 2 * k + b:2 * k + b + 1], None, ALU.mult, None, [], [B("CSR")])
        for n in range(2):
            ss = [load_w("w_ada", g0 + n * 512 + hf * 256, 256) for hf in range(2)]
            for hf in range(2):
                for k in range(8):
                    mm(PS[7][:, hf * 256:(hf + 1) * 256], CSR[:, k, :], WS(ss[hf])[:, k, 0:256], k == 0, k == 7,
                       [B("CSR"), wbuf(ss[hf])], [psb[7]])
            tt("dve", GBC[:, n * 512:(n + 1) * 512], PS[7][:, :], GBC[:, n * 512:(n + 1) * 512],
               ALU.add, [psb[7], B("GBC")], [B("GBC")])
        sc.barrier()

    def layer_norm(b, from_x1, Acol, Bcol0, dstT, dstname, junk):
        for i in range(16):
            sl_ = i % 2
            if from_x1:
                xin = x1[:, i, :]
                xb = B("x1_%d" % (i // 4))
            else:
                xin = XT[:, sl_, :]
                xb = B("XT%d" % sl_)
                dma("sp", XT[:, sl_, :], dr["x"][b, i * 128:(i + 1) * 128, :], writes=[xb])
            act(junk, xin, AF.Square, [xb], [B("junk"), B("ss")], accum_out=ST[:, 0:1])
            act(ST[:, 1:2], ST[:, 0:1], AF.Sqrt, [B("ss")], [B("sd")], bias=EPSC[:, 0:1], scale=1.0 / D)
            recip(ST[:, 2:3], ST[:, 1:2], [B("sd")], [B("rstd")])
            ts("dve", XN[:, sl_, :], xin, ST[:, 2:3], None, ALU.mult, None, [xb, B("rstd")], [B("XN%d" % sl_)])
            pst = PSB[6 + sl_]
            for k in range(8):
                tr(pst[:, k * 128:(k + 1) * 128], XN[:, sl_, k * 128:(k + 1) * 128], [B("XN%d" % sl_)],
                   [psb[6 + sl_]])
            for k in range(8):
                act(dstT[:, k, i * 128:(i + 1) * 128], pst[:, k * 128:(k + 1) * 128], AF.Identity,
                    [psb[6 + sl_]], [B("%s_%d" % (dstname, i // 4))],
                    scale=Acol[:, b * 8 + k:b * 8 + k + 1],
                    bias=modfm[:, b * 48 + Bcol0 + k:b * 48 + Bcol0 + k + 1])

    def proj_fm(c0, norm_gcol, dst, dstbuf, dup64=False):
        if dup64:
            s = next_slot()
            load_w("w_in", c0, 64, slot=s)
            load_w("w_in", c0, 64, slot=s, cdst=64)
        else:
            s = load_w("w_in", c0, 128)
        for n in range(4):
            pn = n % 2
            P1 = PS[pn]
            for k in range(8):
                mm(P1[:, :], WS(s)[:, k, 0:128], hT[:, k, n * 512:(n + 1) * 512], k == 0, k == 7,
                   [wbuf(s), B("hT_%d" % n)], [psb[pn]])
            ob = B("%s_%d" % (dstbuf, n))
            if norm_gcol is None:
                cp("act", dst[:, n * 512:(n + 1) * 512], P1[:, :], [psb[pn]], [ob])
                continue
            act(SQ[:, pn, :], P1[:, :], AF.Square, [psb[pn]], [B("SQ%d" % pn)])
            P2 = PS[2 + pn]
            mm(P2[:, :], bones[:, :], SQ[:, pn, :], True, True, [B("SQ%d" % pn)], [psb[2 + pn]])
            act(RS[:, pn, :], P2[:, :], AF.Ln, [psb[2 + pn]], [B("RS%d" % pn)], bias=EPSC[:, 0:1])
            act(RS[:, pn, :], RS[:, pn, :], AF.Exp, [B("RS%d" % pn)], [B("RS%d" % pn)], scale=-0.5)
            stt(dst[:, n * 512:(n + 1) * 512], P1[:, :], qkg[:, norm_gcol:norm_gcol + 1], RS[:, pn, :],
                ALU.mult, ALU.mult, [psb[pn], B("RS%d" % pn)], [ob])

    def proj_v(c0):
        for hf in range(2):
            s = load_w("w_in", c0 + hf * 256, 256)
            for i in range(16):
                pn = i % 2
                for k in range(8):
                    mm(PS[pn][:, 0:256], hT[:, k, i * 128:(i + 1) * 128], WS(s)[:, k, 0:256], k == 0, k == 7,
                       [wbuf(s), B("hT_%d" % (i // 4))], [psb[pn]])
                cp("act" if i % 2 else "dve", V[:, i, hf * 256:(hf + 1) * 256], PS[pn][:, 0:256], [psb[pn]], [B("V")])

    ptc = [0]
    sc.op("pool", lambda e: e.memset(QZ[:, :, :], 0.0), (), [B("QZ0"), B("QZ1")])

    def attn_runs(runs, hook=None):
        blocks = []
        for r, R in enumerate(runs):
            nst = 4 * R["n"] + 4
            for j in range(nst):
                blocks.append((r, j, nst))

        def prep(r):
            if r >= len(runs):
                return
            R = runs[r]
            zs = R["prow"] // 64
            pr = R["prow"]
            cp("pool", QZ[pr:pr + 64, zs, :], QK[pr:pr + 64, R["qt"], R["n"] * 512:(R["n"] + 1) * 512],
               [B("QK%d_%d" % (R["qt"], R["n"]))], [B("QZ%d" % zs)])

        state = {}

        def stage_a(i):
            r, j, nst = blocks[i]
            R = runs[r]
            n, hh, kt, use_mask = R["n"], R["hh"], R["kt"], R["use_mask"]
            zs = R["prow"] // 64
            qa_ = qaugd if use_mask else qaug
            qab = [B("qaugd")] if use_mask else []
            jd = j - 4 * n
            c_lo = 0 if jd < 0 else 128 * jd
            sb_ = ptc[0] % 2
            pslot = ptc[0] % 3
            ptc[0] += 1
            state[i] = (sb_, pslot, c_lo)
            SP_ = PS[sb_]
            ksl = QK[:, kt, j * 128:(j + 1) * 128]
            kb_, qb_ = B("QK%d_%d" % (kt, j // 4)), B("QZ%d" % zs)
            if jd < 0:
                groups = [(0, 512, False)]
            else:
                groups = [(c_lo, c_lo + 128, True)]
                if c_lo + 128 < 512:
                    groups.append((c_lo + 128, 512, False))
            for (a_, bnd, isdiag) in groups:
                if use_mask:
                    mm(SP_[:, a_:bnd], ksl, QZ[:, zs, a_:bnd], True, False, [kb_, qb_], [psb[sb_]])
                    mm(SP_[:, a_:bnd], kaug[:, hh * 128:(hh + 1) * 128], qa_[:, a_:bnd], False, False, qab, [psb[sb_]])
                else:
                    mm(SP_[:, a_:bnd], ksl, QZ[:, zs, a_:bnd], True, not isdiag, [kb_, qb_], [psb[sb_]])
                if use_mask:
                    mm(SP_[:, a_:bnd], ident[:, :], MBT[:, j, a_:bnd], False, not isdiag, [B("MBT")], [psb[sb_]])
                if isdiag:
                    mm(SP_[:, a_:bnd], ident[:, :], corr[:, hh * 128:(hh + 1) * 128], False, True, [], [psb[sb_]])
            if j == nst - 1:
                prep(r + 2)

        def stage_bc(i):
            r, j, nst = blocks[i]
            R = runs[r]
            n, hh = R["n"], R["hh"]
            sb_, pslot, c_lo = state.pop(i)
            if R["use_mask"]:
                imm = -SLOPES[hh] * (512 * n - 128 * j)
                act(PT[:, pslot, c_lo:512], PS[sb_][:, c_lo:512], AF.Exp, [psb[sb_]], [B("PT%d" % pslot)],
                    bias=IMM[:, immcol(imm):immcol(imm) + 1])
            else:
                for gg in range(c_lo // 256, 2):
                    lo_ = max(c_lo, 256 * gg)
                    dl = j - 4 * n - 2 * gg
                    col = (hh - 8) * 17 + dl + 15
                    act(PT[:, pslot, lo_:256 * gg + 256], PS[sb_][:, lo_:256 * gg + 256], AF.Exp, [psb[sb_]],
                        [B("PT%d" % pslot)], bias=BK[:, col:col + 1])
            ob_, sbk = R["obank"], R["sbank"]
            vc = R["vcols"]
            mm(PS[ob_][:, c_lo:512], V[:, j, vc:vc + 128], PT[:, pslot, c_lo:512], j == 0, j == nst - 1,
               [B("V"), B("PT%d" % pslot)], [psb[ob_]])
            mm(PS[sbk][:, c_lo:512], ones[:, :], PT[:, pslot, c_lo:512], j == 0, j == nst - 1,
               [B("PT%d" % pslot)], [psb[sbk]])
            if j == nst - 1:
                R["fin"]()

        prep(0)
        prep(1)
        stage_a(0)
        for i in range(len(blocks)):
            if i + 1 < len(blocks):
                stage_a(i + 1)
            stage_bc(i)
            if hook is not None:
                hook()

    def act_recip(dst, src, rb, wb):
        act(dst, src, AF.Ln, rb, wb)
        act(dst, dst, AF.Exp, wb, wb, scale=-1.0)

    immvals = sorted({-sp_ * (512 * n - 128 * j) for sp_ in set(SLOPES) for n in range(4) for j in range(4 * n + 4)})
    immidx = {v: i for i, v in enumerate(immvals)}
    IMM = salloc("IMM", [128, len(immvals)], F32)
    assert base[0] <= 229344, base[0]

    def immcol(v):
        return immidx[v]

    for v, i in immidx.items():
        memset(IMM[:, i:i + 1], float(v), [CONST])
    sc.barrier()

    def locals_():
        return dict(hT=hT, QK=QK, V=V, QI=QI, KI=KI, WI=WI, yT=yT, x1=x1, h2T=h2T, mrgT=mrgT, MBT=MBT,
                    IDX=IDX, modfm=modfm, GBC=GBC, BIS=BIS, A1=A1, nlam=nlam, MB=MB)

    if stop_after == "p0":
        nb = 0
    for b in range(nb):
        layer_norm(b, False, A1, 0, hT, "hT", JUNKA[:, 0:D])
        sc.barrier()
        if stop_after == "ln1":
            break

        set_slots([0, 1, 2, 3, 4, 5])
        for p in range(4):
            proj_fm(C_QA + p * 128, 0, QK[:, p, :], "QK%d" % p)
            proj_fm(C_KA + p * 128, 1, QK[:, 4 + p, :], "QK%d" % (4 + p))
        proj_v(C_VA)
        for p in range(2):
            proj_fm(C_QI + p * 128, None, QI[:, p, :], "QI")
        proj_fm(C_KI, None, KI[:, :], "KI", dup64=True)
        s = load_w("w_in", C_WI, 4)
        for i in range(16):
            for k in range(8):
                mm(PS[7][:, i * 4:(i + 1) * 4], hT[:, k, i * 128:(i + 1) * 128], WS(s)[:, k, 0:4], k == 0, k == 7,
                   [wbuf(s), B("hT_%d" % (i // 4))], [psb[7]])
        cp("dve", WI[:, 0:64], PS[7][:, 0:64], [psb[7]], [B("WI")])
        sc.barrier()
        set_slots([0, 1])
        if stop_after == "proj_a":
            break

        dma("sp", TIEF[:, :], dr["tief"], writes=[B("TIEF")])
        memset(DMINX[:, :, :], 0.0, [B("DMINX")])
        MBs = [MB[:, 0, :], MB[:, 1, :], WB[:, 0, :, :].rearrange("p k c -> p (k c)"),
               WB[:, 1, :, :].rearrange("p k c -> p (k c)")]

        def maskA(n):
            for half in range(2):
                tiles = [4 * n + 2 * half, 4 * n + 2 * half + 1]
                for gi, i in enumerate(tiles):
                    N = (i + 1) * 128
                    idxb = B("IDX%d" % gi)
                    for jb in range((N + 511) // 512):
                        cols = min(512, N - jb * 512)
                        for h in range(4):
                            pr = (h % 2) * 64
                            pb = 2 + (h % 2)
                            mm(PS[pb][:, 0:cols], QI[pr:pr + 64, h // 2, i * 128:(i + 1) * 128],
                               KI[pr:pr + 64, jb * 512:jb * 512 + cols], True, True, [B("QI"), B("KI")], [psb[pb]])
                            rs_ = h % 2
                            act(T1[:, rs_, 0:cols], PS[pb][:, 0:cols], AF.Relu, [psb[pb]], [B("T1%d" % rs_)])
                            dst = IDX[:, gi, jb * 512:jb * 512 + cols]
                            if h == 0:
                                ts("dve", dst, T1[:, rs_, 0:cols], WI[:, i * 4:i * 4 + 1], None, ALU.mult, None,
                                   [B("T1%d" % rs_), B("WI")], [idxb])
                            else:
                                stt(dst, T1[:, rs_, 0:cols], WI[:, i * 4 + h:i * 4 + h + 1], dst, ALU.mult, ALU.add,
                                    [B("T1%d" % rs_), B("WI"), idxb], [idxb])
                        yield
                    tt("dve", IDX[:, gi, i * 128:(i + 1) * 128], IDX[:, gi, i * 128:(i + 1) * 128], negblk[:, :],
                       ALU.add, [idxb], [idxb])
                    if i >= 2:
                        stt(IDX[:, gi, 0:N], IDX[:, gi, 0:N], 0.0, IDX[:, gi, 0:N], ALU.is_ge, ALU.add, [idxb], [idxb])
                        ts("dve", JUNK8[:, 0:N], IDX[:, gi, 0:N], 1.0, None, ALU.is_equal, None, [idxb], [B("junk")])
                        sc.op("dve", lambda e, gi=gi, N=N: e.copy_predicated(IDX[:, gi, 0:N], JUNK8[:, 0:N], TIEF[:, 0:N]),
                              [B("junk"), B("TIEF")], [idxb])
                    yield
                bis = B("BIS")
                if tiles[0] >= 2:
                    for gi, i in enumerate(tiles):
                        N = (i + 1) * 128
                        red(BIS[:, gi:gi + 1], IDX[:, gi, 0:N], ALU.max, [B("IDX%d" % gi)], [bis])
                        red(BIS[:, 2 + gi:3 + gi], IDX[:, gi, 0:N - 64], ALU.min, [B("IDX%d" % gi)], [bis])
                    stt(BIS[:, 4:6], BIS[:, 0:2], 1.0, BIS[:, 2:4], ALU.add, ALU.subtract, [bis], [bis])
                    yield
                    for it in range(NBIS):
                        stt(BIS[:, 6:8], BIS[:, 4:6], 2.0 ** (-(it + 1)), BIS[:, 2:4], ALU.mult, ALU.add, [bis], [B("MID")])
                        N0 = (tiles[0] + 1) * 128
                        N1 = (tiles[1] + 1) * 128
                        ts("dve", JUNK8[:, 0:N0], IDX[:, 0, 0:N0], BIS[:, 6:7], 0.0, ALU.is_ge, ALU.add,
                           [B("IDX0"), B("MID")], [B("junk"), B("CNT0")], accum_out=BIS[:, 8:9])
                        act(JUNK2[:, 0:N1], IDX[:, 1, 0:N1], AF.Sign, [B("IDX1"), B("MID")], [B("junk2"), B("CNT1")],
                            bias=BIS[:, 7:8], scale=-1.0, accum_out=BIS[:, 9:10])
                        ts("dve", BISU[:, 0:1], BIS[:, 8:9], float(TOPK), None, ALU.is_ge, None,
                           [B("CNT0")], [B("BD")])
                        ts("dve", BISU[:, 1:2], BIS[:, 9:10], float(N1) - 511.5, None, ALU.is_le, None,
                           [B("CNT1")], [B("BD")])
                        sc.op("dve", lambda e: e.copy_predicated(BIS[:, 2:4], BISU[:, 0:2], BIS[:, 6:8]),
                              [B("BD"), B("MID")], [bis])
                        yield
                else:
                    memset(BIS[:, 2:4], -1e29, [bis])
                for gi, i in enumerate(tiles):
                    N = (i + 1) * 128
                    g4 = i - 4 * n
                    mbt_ = MBs[g4]
                    ts("dve", mbt_[:, 0:N], IDX[:, gi, 0:N], BIS[:, 2 + gi:3 + gi], NEG, ALU.is_lt, ALU.mult,
                       [B("IDX%d" % gi), bis], [B("MB%d" % g4)])
                    tt("dve", IDX[:, gi, 0:N], mbt_[:, 0:N], ndist[:, 1920 - 128 * i:1920 - 128 * i + N], ALU.add,
                       [B("MB%d" % g4), B("IDX%d" % gi)], [B("IDX%d" % gi)])
                    red(DMIN[:, g4:g4 + 1], IDX[:, gi, 0:N], ALU.max, [B("IDX%d" % gi)], [B("DMIN")])
                    cp("dve", DMINX[:, g4, g4:g4 + 1], DMIN[:, g4:g4 + 1], [B("DMIN")], [B("DMINX")])
                    yield

        def maskB(n):
            for g4 in range(4):
                i = 4 * n + g4
                for j0 in range(0, i + 1, 4):
                    js = list(range(j0, min(j0 + 4, i + 1)))
                    pb = 4 + ((j0 // 4) % 2)
                    for jj, j in enumerate(js):
                        tr(PSB[pb][:, jj * 128:(jj + 1) * 128], MBs[g4][:, j * 128:(j + 1) * 128],
                           [B("MB%d" % g4)], [psb[pb]])
                    for jj, j in enumerate(js):
                        cp("act" if jj % 2 else "dve", MBT[:, j, g4 * 128:(g4 + 1) * 128],
                           PSB[pb][:, jj * 128:(jj + 1) * 128], [psb[pb]], [B("MBT")])
            for g4 in range(4):
                tr(PSB[4][0:4, g4 * 128:(g4 + 1) * 128], DMINX[:, g4, 0:4], [B("DMINX")], [psb[4]])
            cp("dve", qaugd[0:4, 0:512], PSB[4][0:4, 0:512], [psb[4]], [B("qaugd")])

        for _ in maskA(0):
            pass
        maskB(0)
        for n in range(4):
            gen = maskA(n + 1) if n < 3 else iter(())
            nblk = 8 * (4 * n + 4)
            nyield = 2 * (NBIS + 12) if n < 3 else 0
            stepacc = [0.0]

            def hook(gen=gen, per=nyield / float(nblk)):
                stepacc[0] += per
                while stepacc[0] >= 1.0:
                    stepacc[0] -= 1.0
                    next(gen, None)

            runs = []
            for h in range(8):
                ob, sb2 = (4, 5) if h % 2 == 0 else (6, 7)
                pr = (h % 2) * 64

                def fin(h=h, ob=ob, sb2=sb2, pr=pr, n=n):
                    rsl = h % 2
                    act_recip(RS[pr:pr + 64, rsl, :], PS[sb2][pr:pr + 64, :], [psb[sb2]], [B("RS%d" % rsl)])
                    tt("dve", yT[pr:pr + 64, h // 2, n * 512:(n + 1) * 512], PS[ob][pr:pr + 64, :], RS[pr:pr + 64, rsl, :],
                       ALU.mult, [psb[ob], B("RS%d" % rsl)], [B("yT_%d" % n)])
                runs.append(dict(n=n, hh=h, qt=h // 2, kt=4 + h // 2, prow=pr, vcols=(h // 2) * 128, obank=ob, sbank=sb2,
                                 use_mask=True, fin=fin))
            attn_runs(runs, hook)
            for _ in gen:
                pass
            if n < 3:
                maskB(n + 1)
        sc.barrier()
        if stop_after in ("dsa", "mask"):
            break

        set_slots([0, 1, 2, 3, 4, 5])
        for p in range(4):
            proj_fm(C_QB + p * 128, 2, QK[:, p, :], "QK%d" % p)
            proj_fm(C_KB + p * 128, 3, QK[:, 4 + p, :], "QK%d" % (4 + p))
        proj_v(C_VB)
        set_slots([0, 1])
        for n in range(4):
            runs = []
            for h in range(4):
                def fin1():
                    recip(T1[:, 0, :], PS[5][:, :], [psb[5]], [B("T10")])
                    tt("dve", RS[:, 0, :], PS[4][:, :], T1[:, 0, :], ALU.mult, [psb[4], B("T10")], [B("RS0")])

                def fin2(h=h, n=n):
                    recip(T1[:, 1, :], PS[7][:, :], [psb[7]], [B("T11")])
                    tt("dve", RS[:, 1, :], PS[6][:, :], T1[:, 1, :], ALU.mult, [psb[6], B("T11")], [B("RS1")])
                    stt(RS[:, 0, :], RS[:, 1, :], nlam[:, 0:1], RS[:, 0, :], ALU.mult, ALU.add,
                        [B("RS0"), B("RS1")], [B("RS0")])
                    act(SQ[:, 0, :], RS[:, 0, :], AF.Square, [B("RS0")], [B("SQ0")])
                    mm(PS[2][:, :], omean[:, :], SQ[:, 0, :], True, True, [B("SQ0")], [psb[2]])
                    act(T1[:, 0, :], PS[2][:, :], AF.Ln, [psb[2]], [B("T10")], bias=EPSC[:, 0:1])
                    act(T1[:, 0, :], T1[:, 0, :], AF.Exp, [B("T10")], [B("T10")], scale=-0.5)
                    stt(yT[:, 4 + h, n * 512:(n + 1) * 512], RS[:, 0, :], subg[:, 0:1], T1[:, 0, :], ALU.mult, ALU.mult,
                        [B("RS0"), B("T10")], [B("yT_%d" % n)])
                runs.append(dict(n=n, hh=8 + h, qt=h, kt=4 + h, prow=0, vcols=h * 128, obank=4, sbank=5,
                                 use_mask=False, fin=fin1))
                runs.append(dict(n=n, hh=8 + h, qt=h, kt=4 + h, prow=64, vcols=h * 128, obank=6, sbank=7,
                                 use_mask=False, fin=fin2))
            attn_runs(runs)
        sc.barrier()
        if stop_after == "diff":
            break

        set_slots([0, 1, 2, 3, 4, 5])
        for c in range(8):
            sga = load_w("w_in", C_G + c * 128, 128)
            sgb = load_w("w_in", C_G + D + c * 128, 128)
            for n in range(4):
                for gsel, sg in enumerate([sga, sgb]):
                    for k in range(8):
                        mm(PS[gsel][:, :], WS(sg)[:, k, 0:128], hT[:, k, n * 512:(n + 1) * 512], k == 0, k == 7,
                           [wbuf(sg), B("hT_%d" % n)], [psb[gsel]])
                    act(GTf[:, gsel, n * 512:(n + 1) * 512], PS[gsel][:, :], AF.Sigmoid, [psb[gsel]],
                        [B("GT%d_%d" % (gsel, n))])
            su = next_slot()
            load_w("w_up_a", c * 128, 128, 0, 4, slot=su, kdst=0)
            load_w("w_up_b", c * 128, 128, 0, 4, slot=su, kdst=4)
            for n in range(4):
                for kk in range(4):
                    mm(PS[2][:, :], WS(su)[:, kk, 0:128], yT[:, kk, n * 512:(n + 1) * 512], kk == 0, kk == 3,
                       [wbuf(su), B("yT_%d" % n)], [psb[2]])
                for kk in range(4):
                    mm(PS[3][:, :], WS(su)[:, 4 + kk, 0:128], yT[:, 4 + kk, n * 512:(n + 1) * 512], kk == 0, kk == 3,
                       [wbuf(su), B("yT_%d" % n)], [psb[3]])
                tt("dve", T1[:, 0, :], PS[2][:, :], GTf[:, 0, n * 512:(n + 1) * 512], ALU.mult,
                   [psb[2], B("GT0_%d" % n)], [B("T10")])
                tt("dve", T1[:, 1, :], PS[3][:, :], GTf[:, 1, n * 512:(n + 1) * 512], ALU.mult,
                   [psb[3], B("GT1_%d" % n)], [B("T11")])
                tt("pool", mrgT[:, c, n * 512:(n + 1) * 512], T1[:, 0, :], T1[:, 1, :], ALU.add,
                   [B("T10"), B("T11")], [B("mrgT_%d" % n)])
        sc.barrier()
        set_slots([0, 1])

        gate_bc(b, 0)
        for q4 in range(4):
            dma("pool", Wo[:, :, q4 * 256:(q4 + 1) * 256], w3("w_o", q4 * 256, 256), writes=[B("Wo")])
        for i in range(16):
            dma("sp", x1[:, i, :], dr["x"][b, i * 128:(i + 1) * 128, :], writes=[B("x1_%d" % (i // 4))])
        for i in range(16):
            for dd in range(2):
                for c in range(8):
                    mm(PS[dd][:, :], mrgT[:, c, i * 128:(i + 1) * 128], Wo[:, c, dd * 512:(dd + 1) * 512], c == 0, c == 7,
                       [B("mrgT_%d" % (i // 4)), B("Wo")], [psb[dd]])
                tt("dve", T1[:, dd, :], PS[dd][:, :], GBC[:, dd * 512:(dd + 1) * 512], ALU.mult,
                   [psb[dd]], [B("T1%d" % dd)])
                tt("pool", x1[:, i, dd * 512:(dd + 1) * 512], x1[:, i, dd * 512:(dd + 1) * 512], T1[:, dd, :], ALU.add,
                   [B("T1%d" % dd), B("x1_%d" % (i // 4))], [B("x1_%d" % (i // 4))])
        sc.barrier()
        if stop_after == "x1":
            break

        layer_norm(b, True, A2, 24, h2T, "h2T", JUNKE[:, 0:D])
        sc.barrier()

        gate_bc(b, 1)
        for th in range(2):
            set_slots([0, 1, 6, 7, 8, 9])
            for fp in range(11):
                sa = load_w("w_ff1", fp * 256, 256)
                sb = load_w("w_ff3", fp * 256, 256)
                for ff in range(2):
                    f = 2 * fp + ff
                    for nn in range(2):
                        n = 2 * th + nn
                        p0 = 2 * ((2 * ff + nn) % 2)
                        for k in range(8):
                            mm(PS[p0][:, :], WS(sa)[:, k, ff * 128:(ff + 1) * 128], h2T[:, k, n * 512:(n + 1) * 512],
                               k == 0, k == 7, [wbuf(sa), B("h2T_%d" % n)], [psb[p0]])
                        for k in range(8):
                            mm(PS[p0 + 1][:, :], WS(sb)[:, k, ff * 128:(ff + 1) * 128], h2T[:, k, n * 512:(n + 1) * 512],
                               k == 0, k == 7, [wbuf(sb), B("h2T_%d" % n)], [psb[p0 + 1]])
                        sq = (2 * ff + nn) % 2
                        act(SQ[:, sq, :], PS[p0][:, :], AF.Silu, [psb[p0]], [B("SQ%d" % sq)])
                        tt("dve", uT[:, f, nn * 512:(nn + 1) * 512], PS[p0 + 1][:, :], SQ[:, sq, :], ALU.mult,
                           [psb[p0 + 1], B("SQ%d" % sq)], [B("uT")])
            sc.barrier()
            set_slots([0, 1])
            for dq in range(4):
                dma("pool", W2[:, :, :], dr["w_ff2"].rearrange("(f p) d -> p f d", p=128)[:, :, dq * 256:(dq + 1) * 256],
                    writes=[B("W2")])
                for g in range(8):
                    pg = 4 + (g % 2)
                    i = 8 * th + g
                    for f in range(22):
                        mm(PS[pg][:, 0:256], uT[:, f, g * 128:(g + 1) * 128], W2[:, f, :], f == 0, f == 21,
                           [B("uT"), B("W2")], [psb[pg]])
                    tt("dve", T1[:, g % 2, 0:256], PS[pg][:, 0:256], GBC[:, dq * 256:(dq + 1) * 256], ALU.mult,
                       [psb[pg]], [B("T1%d" % (g % 2))])
                    tt("pool", x1[:, i, dq * 256:(dq + 1) * 256], x1[:, i, dq * 256:(dq + 1) * 256], T1[:, g % 2, 0:256],
                       ALU.add, [B("T1%d" % (g % 2)), B("x1o_%d" % i)], [B("x1o_%d" % i)])
            for g in range(8):
                i = 8 * th + g
                dma("sp", out_d[b, i * 128:(i + 1) * 128, :], x1[:, i, :], reads=[B("x1o_%d" % i)], writes=[B("outd")])
            sc.barrier()

    loc = locals_()
    for name, v in dbg.items():
        dma("sp", dbg_d[name], v[0](loc), writes=[B("dbgout")])
    sc.barrier()

    sems = {e: nc.alloc_semaphore("s_" + e) for e in ("pe", "act", "dve", "pool")}
    dsems = {"sp": [nc.alloc_semaphore("dsp%d" % i) for i in range(8)],
             "pool": [nc.alloc_semaphore("dpl%d" % i) for i in range(6)]}
    sc.prepare(sems, dsems)
    stats = {}
    with nc.Block() as block:

        @block.tensor
        def _(e):
            stats["pe"] = sc.emit_engine("pe", e)

        @block.scalar
        def _(e):
            stats["act"] = sc.emit_engine("act", e)

        @block.vector
        def _(e):
            stats["dve"] = sc.emit_engine("dve", e)

        @block.gpsimd
        def _(e):
            stats["pool"] = sc.emit_engine("pool", e)

        @block.sync
        def _(e):
            stats["sp"] = sc.emit_engine("sp", e)

    return nc, stats


def _core_inputs(inp, core):
    f = np.float32
    g = lambda k: np.asarray(inp[k], dtype=f)
    b0 = core * NB
    c = g("c")[b0:b0 + NB]
    c_fm = np.ascontiguousarray(c.reshape(NB, 8, 128).transpose(2, 1, 0).reshape(128, 16))
    b_ada = g("b_ada")[0]
    m = dict(
        x=np.ascontiguousarray(g("x")[b0:b0 + NB]),
        c_fm=c_fm,
        w_ada=np.ascontiguousarray(g("w_ada")[0]),
        b_ada_fm=np.ascontiguousarray(b_ada.reshape(48, 128).T),
        bg=np.ascontiguousarray(np.broadcast_to(np.concatenate([b_ada[2048:3072], b_ada[5120:6144]])[None, :], (128, 2048))),
        n1g=np.ascontiguousarray(g("norm1_g")[0].reshape(8, 128).T),
        n2g=np.ascontiguousarray(g("norm2_g")[0].reshape(8, 128).T),
        w_in=np.ascontiguousarray(g("w_in")[0]),
        qkg=np.ascontiguousarray(np.stack([np.tile(g("qn_a")[0], 2), np.tile(g("kn_a")[0], 2),
                                           np.tile(g("qn_b")[0], 2), np.tile(g("kn_b")[0], 2)], axis=1)),
        subg=np.ascontiguousarray(g("subln_g")[0].reshape(128, 1)),
        lamv=np.ascontiguousarray(np.concatenate([g("lam_q1")[0], g("lam_k1")[0], g("lam_q2")[0], g("lam_k2")[0]])[None, :]),
        w_up_a=np.ascontiguousarray(g("w_up_a")[0]), w_up_b=np.ascontiguousarray(g("w_up_b")[0]),
        w_o=np.ascontiguousarray(g("w_o")[0]), w_ff1=np.ascontiguousarray(g("w_ff1")[0]),
        w_ff3=np.ascontiguousarray(g("w_ff3")[0]), w_ff2=np.ascontiguousarray(g("w_ff2")[0]),
    )
    return m


_CACHE = {}


def kernel(**inputs):
    consts = _consts()
    shared = None
    in_maps = []
    for core in range(NCORES):
        m = _core_inputs(inputs, core)
        if shared is None:
            shared = {k: m[k] for k in m if k not in ("x", "c_fm")}
        else:
            for k in shared:
                m[k] = shared[k]
        m.update(consts)
        in_maps.append(m)
    if "nc" not in _CACHE:
        _CACHE["nc"] = build()[0]
    nc = _CACHE["nc"]
    res = run_bass_kernel_spmd(nc, in_maps, core_ids=list(range(NCORES)))
    outs = [np.asarray(r["out"], dtype=np.float32) for r in res.results]
    return np.concatenate(outs, axis=0)
```

```python
import numpy as np
import ml_dtypes
import concourse.bass as bass
import concourse.mybir as mybir
from concourse.bass_utils import run_bass_kernel_spmd

F32 = mybir.dt.float32
BF16 = mybir.dt.bfloat16
ALU = mybir.AluOpType
AF = mybir.ActivationFunctionType
AX = mybir.AxisListType

S = 2048
D = 1024
DFF = 2816
NB = 2
NCORES = 8
W_IN = 5444
C_QA, C_KA, C_VA, C_QB, C_KB, C_VB, C_QI, C_KI, C_WI, C_G = 0, 512, 1024, 1536, 2048, 2560, 3072, 3328, 3392, 3396
NEG = -30000.0
TOPK = 256
NBIS = 26
EPS = 1e-6
SLOPES = [2.0 ** (-(h + 1)) for h in range(8)] + [2.0 ** (-2 * (h + 1)) for h in range(4)]


class Buf:
    __slots__ = ("name", "w", "r")

    def __init__(self, name):
        self.name = name
        self.w = {}
        self.r = {}


class Op:
    __slots__ = ("eng", "fn", "deps", "need", "cnt", "dma", "sem", "semval", "prevval")


class Sched:
    ENGS = ("pe", "act", "dve", "pool", "sp")

    def __init__(self):
        self.q = {e: [] for e in self.ENGS}
        self.bufs = {}
        self.dmas = []
        self.all_dmas = []

    def B(self, name):
        b = self.bufs.get(name)
        if b is None:
            b = self.bufs[name] = Buf(name)
        return b

    def _dep(self, o, t, hazard, strict=False):
        if t is o:
            return
        if not o.dma and not t.dma and t.eng == o.eng:
            if o.eng == "pe" or (hazard != "raw" and not strict):
                return
        o.deps.append(t)

    def op(self, eng, fn, reads=(), writes=(), dma=False):
        o = Op()
        o.eng, o.fn, o.dma, o.deps, o.need = eng, fn, dma, [], False
        o.cnt = 0
        for b in reads:
            for w in b.w.values():
                self._dep(o, w, "raw")
        for b in writes:
            st = b.name.startswith("junk")
            for w in b.w.values():
                self._dep(o, w, "waw", st)
            for r in b.r.values():
                self._dep(o, r, "war", st)
        for b in reads:
            b.r[id(o) if dma else eng] = o
        for b in writes:
            b.w = {(id(o) if dma else eng): o}
            b.r = {}
        self.q[eng].append(o)
        if dma:
            self.dmas.append(o)
            self.all_dmas.append(o)
        return o

    def barrier(self):
        lasts = []
        for e in self.ENGS:
            for o in reversed(self.q[e]):
                if o.fn is not None and not o.dma:
                    lasts.append(o)
                    break
        for e in self.ENGS:
            o = Op()
            o.eng, o.fn, o.dma, o.need, o.cnt = e, None, False, False, 0
            o.deps = [t for t in lasts if t.eng != e] + list(self.dmas)
            self.q[e].append(o)
        self.dmas = []
        for b in self.bufs.values():
            b.w = {}
            b.r = {}

    def prepare(self, sems, dma_sems):
        for e in self.ENGS:
            for o in self.q[e]:
                for t in o.deps:
                    t.need = True
        for e in self.ENGS:
            cnt = 0
            pool = dma_sems.get(e, [])
            uses = [0] * len(pool)
            rr = 0
            for o in self.q[e]:
                if o.dma:
                    i = rr % len(pool)
                    rr += 1
                    o.sem = pool[i]
                    o.prevval = 16 * uses[i]
                    uses[i] += 1
                    o.semval = 16 * uses[i]
                elif o.need:
                    cnt += 1
                    o.cnt = cnt
                    o.sem = sems[e]
                    o.semval = cnt

    def emit_engine(self, e, eng):
        waited = {}
        nw = 0
        ni = 0
        for o in self.q[e]:
            req = {}
            for t in o.deps:
                key = id(t.sem)
                if key not in req or req[key][1] < t.semval:
                    req[key] = (t.sem, t.semval)
            if o.dma and o.prevval > 0:
                key = id(o.sem)
                if key not in req or req[key][1] < o.prevval:
                    req[key] = (o.sem, o.prevval)
            for key, (sem, val) in req.items():
                if waited.get(key, 0) < val:
                    eng.wait_ge(sem, val)
                    waited[key] = val
                    nw += 1
            if o.fn is None:
                continue
            inst = o.fn(eng)
            ni += 1
            if o.dma:
                inst.then_inc(o.sem, 16)
            elif o.need:
                inst.then_inc(o.sem, 1)
        return (ni, nw)


def _consts():
    bf = ml_dtypes.bfloat16
    ident = np.eye(128, dtype=np.float32).astype(bf)
    bones = np.zeros((128, 128), np.float32)
    bones[:64, :64] = 1.0 / 64
    bones[64:, 64:] = 1.0 / 64
    omean = np.full((128, 128), 1.0 / 128, np.float32)
    ones = np.ones((128, 128), np.float32)
    tl = np.arange(512)
    qaug = np.zeros((128, 512), np.float32)
    qaug[4], qaug[5], qaug[6] = tl // 64, tl % 64, 1.0
    sl = np.arange(128)
    kaug = np.zeros((128, 12 * 128), np.float32)
    corr = np.zeros((128, 12 * 128), np.float32)
    for h, sp in enumerate(SLOPES):
        kaug[0:4, h * 128:(h + 1) * 128] = -sp
        kaug[4, h * 128:(h + 1) * 128] = -64.0 * sp
        kaug[5, h * 128:(h + 1) * 128] = -sp
        kaug[6, h * 128:(h + 1) * 128] = sp * sl
        ss, tt = np.meshgrid(sl, sl, indexing="ij")
        c = np.where((ss // 64) > (tt // 64), NEG,
                     np.where(ss > tt, -2.0 * sp * (ss - tt), 0.0))
        corr[:, h * 128:(h + 1) * 128] = c
    negblk = np.zeros((128, 128), np.float32)
    negblk[:64, 64:] = -1e30
    bk = np.zeros((128, 68), np.float32)
    for hd in range(4):
        for dl in range(-15, 2):
            bk[:, hd * 17 + dl + 15] = SLOPES[8 + hd] * (np.arange(128) + 128.0 * dl - 255.0)
    pp, cc = np.meshgrid(np.arange(128), np.arange(2048), indexing="ij")
    ndist = (-np.abs(pp + 1920 - cc)).astype(np.float32)
    tief = np.ascontiguousarray(np.broadcast_to((1.0 - (np.arange(2048) + 1.0) * 2.0 ** -12).astype(np.float32)[None, :], (128, 2048)))
    return dict(ident=ident, bones=bones.astype(bf), omean=omean.astype(bf), ones=ones.astype(bf),
                qaug=qaug.astype(bf), kaug=kaug.astype(bf), corr=corr.astype(bf), negblk=negblk, tief=tief, ndist=ndist.astype(bf), bk=bk)


CONST_SPECS = dict(ident=([128, 128], BF16), bones=([128, 128], BF16), omean=([128, 128], BF16),
                   ones=([128, 128], BF16), qaug=([128, 512], BF16), kaug=([128, 1536], BF16), ndist=([128, 2048], BF16),
                   corr=([128, 1536], BF16), negblk=([128, 128], F32),
                   tief=([128, 2048], F32), bk=([128, 68], F32))

IN_SPECS = dict(
    x=([NB, S, D], F32), c_fm=([128, 16], F32), w_ada=([D, 6 * D], F32), b_ada_fm=([128, 48], F32),
    bg=([128, 2048], F32), n1g=([128, 8], F32), n2g=([128, 8], F32), w_in=([D, W_IN], F32),
    qkg=([128, 4], F32), subg=([128, 1], F32), lamv=([1, 256], F32),
    w_up_a=([512, D], F32), w_up_b=([512, D], F32), w_o=([D, D], F32),
    w_ff1=([D, DFF], F32), w_ff3=([D, DFF], F32), w_ff2=([DFF, D], F32))


def build(nb=NB, dbg=None, stop_after=None):
    nc = bass.Bass("TRN2", target_bir_lowering=False)
    dr = {}
    for k, (shp, dt) in IN_SPECS.items():
        dr[k] = nc.dram_tensor(k, list(shp), dt, kind="ExternalInput").ap()
    for k, (shp, dt) in CONST_SPECS.items():
        dr[k] = nc.dram_tensor(k, list(shp), dt, kind="ExternalInput").ap()
    out_d = nc.dram_tensor("out", [NB, S, D], F32, kind="ExternalOutput").ap()
    dbg = dbg or {}
    dbg_d = {k: nc.dram_tensor("dbg_" + k, list(v[1]), v[2], kind="ExternalOutput").ap()
             for k, v in dbg.items()}

    sc = Sched()
    B = sc.B
    KB = 1024
    base = [16512]

    def region(nbytes):
        at = base[0]
        base[0] += nbytes
        return at

    def at(name, shape, dt, off):
        return nc.alloc_sbuf_tensor_at(name, list(shape), dt, offset=off)

    def salloc(name, shape, dt):
        nbytes = int(np.prod(shape[1:])) * (4 if dt == F32 else 2)
        nbytes = (nbytes + 63) // 64 * 64
        return at(name, shape, dt, region(nbytes))

    A0 = region(32 * KB)
    D0 = region(32 * KB)
    B0 = region(32 * KB)
    C0 = region(16 * KB)
    E0 = region(44 * KB)
    hT = at("hT", [128, 8, S], BF16, A0)
    x1 = at("x1", [128, 16, D], F32, A0)
    yT = at("yT", [128, 8, S], BF16, D0)
    MB = at("MB", [128, 2, S], BF16, D0 + 16 * KB)
    TIEF = at("TIEF", [128, S], F32, D0 + 24 * KB)
    QK = at("QK", [128, 8, S], BF16, B0)
    mrgT = at("mrgT", [128, 8, S], BF16, B0)
    h2T = at("h2T", [128, 8, S], BF16, B0)
    V = at("V", [128, 16, 512], BF16, C0)
    Wo = at("Wo", [128, 8, D], BF16, C0)
    IDX = at("IDX", [128, 2, S], F32, E0)
    MBT = at("MBT", [128, 16, 512], BF16, E0 + 16 * KB)
    QI = at("QI", [128, 2, S], BF16, E0 + 32 * KB)
    KI = at("KI", [128, S], BF16, E0 + 40 * KB)
    XT = at("XT", [128, 2, D], F32, E0)
    XN = at("XN", [128, 2, D], BF16, E0 + 8 * KB)
    JUNKE = at("JUNKE", [128, D], BF16, E0 + 12 * KB)
    CSR = at("CSR", [128, 8, 128], BF16, E0 + 16 * KB)
    GTf = at("GTf", [128, 2, S], BF16, E0)
    uT = at("uT", [128, 22, 1024], BF16, C0)
    W2 = at("W2", [128, 22, 256], BF16, C0 + 44 * KB)
    w2bufs = [at("W2a", [128, 22, 128], BF16, C0 + 44 * KB), at("W2b", [128, 22, 128], BF16, C0 + 44 * KB + 5632)]
    NSLOT = 2
    WB = salloc("WB", [128, NSLOT, 8, 256], BF16)
    JUNK8 = salloc("JUNK8", [128, S], mybir.dt.uint8)
    JUNKA = nc.alloc_sbuf_tensor_at("JUNKA", [128, D], BF16, offset=base[0] - 2048)
    BISU = salloc("BISU", [128, 2], mybir.dt.uint32)
    JUNK2 = salloc("JUNK2", [128, S], mybir.dt.uint8)
    PT0 = base[0]
    PT = salloc("PT", [128, 3, 512], BF16)

    def PT_at(name, shape, dt, off):
        return at(name, shape, dt, PT0 + off)

    SQ = salloc("SQ", [128, 2, 512], BF16)
    RS = salloc("RS", [128, 2, 512], F32)
    T1 = salloc("T1", [128, 2, 512], F32)
    GBC = salloc("GBC", [128, D], F32)
    ident = salloc("ident", [128, 128], BF16)
    bones = salloc("bones", [128, 128], BF16)
    omean = salloc("omean", [128, 128], BF16)
    ones = salloc("ones", [128, 128], BF16)
    qaug = salloc("qaug", [128, 512], BF16)
    qaugd = salloc("qaugd", [128, 512], BF16)
    kaug = salloc("kaug", [128, 1536], BF16)
    QZ = salloc("QZ", [128, 2, 512], BF16)
    ndist = salloc("ndist", [128, S], BF16)
    DMIN = salloc("DMIN", [128, 4], F32)
    BK = salloc("BK", [128, 68], F32)
    DMINX = salloc("DMINX", [128, 4, 4], BF16)
    corr = salloc("corr", [128, 1536], BF16)
    negblk = salloc("negblk", [128, 128], F32)
    cfm = salloc("cfm", [128, 16], F32)
    csT = salloc("csT", [128, 16], BF16)
    bfm = salloc("bfm", [128, 48], F32)
    modfm = salloc("modfm", [128, 96], F32)
    n1g = salloc("n1g", [128, 8], F32)
    n2g = salloc("n2g", [128, 8], F32)
    A1 = salloc("A1", [128, 16], F32)
    A2 = salloc("A2", [128, 16], F32)
    qkg = salloc("qkg", [128, 4], F32)
    subg = salloc("subg", [128, 1], F32)
    lamv = PT_at("lamv", [1, 256], F32, 0)
    lsc = PT_at("lsc", [1, 16], F32, 1024)
    onesf = PT_at("onesf", [1, 128], F32, 1152)
    nlam = salloc("nlam", [128, 1], F32)
    WI = salloc("WI", [128, 64], F32)
    ST = salloc("ST", [128, 16], F32)
    BIS = salloc("BIS", [128, 16], F32)
    EPSC = salloc("EPSC", [128, 1], F32)
    assert base[0] <= 229344, base[0]
    print("sbuf free bytes/partition:", 229344 - base[0])

    PS = [nc.alloc_psum_tensor("ps%d" % i, [128, 512], F32) for i in range(8)]
    PSB = [p.bitcast(BF16) for p in PS]
    psb = [B("ps%d" % i) for i in range(8)]
    CONST = B("const")

    def dma(q, out, in_, reads=(), writes=()):
        return sc.op(q, lambda e: e.dma_start(out=out, in_=in_), reads, writes, dma=True)

    def mm(out, lhsT, rhs, start, stop, reads, writes):
        return sc.op("pe", lambda e: e.matmul(out, lhsT, rhs, start=start, stop=stop), reads, writes)

    def tr(out, in_, reads, writes):
        return sc.op("pe", lambda e: e.transpose(out, in_, ident[:, :]), list(reads) + [CONST], writes)

    def act(out, in_, func, reads, writes, bias=None, scale=None, accum_out=None):
        kw = {}
        if bias is not None:
            kw["bias"] = bias
        if scale is not None:
            kw["scale"] = scale
        if accum_out is not None:
            kw["accum_out"] = accum_out
        return sc.op("act", lambda e: e.activation(out, in_, func, **kw), reads, writes)

    def ts(eng, out, in0, s1, s2, op0, op1, reads, writes, accum_out=None):
        kw = {}
        if accum_out is not None:
            kw["accum_out"] = accum_out
        if op1 is None:
            return sc.op(eng, lambda e: e.tensor_scalar(out, in0, s1, None, op0, **kw), reads, writes)
        return sc.op(eng, lambda e: e.tensor_scalar(out, in0, s1, s2, op0, op1, **kw), reads, writes)

    def stt(out, in0, scalar, in1, op0, op1, reads, writes):
        return sc.op("dve", lambda e: e.scalar_tensor_tensor(out, in0, scalar, in1, op0, op1), reads, writes)

    def tt(eng, out, in0, in1, op, reads, writes):
        return sc.op(eng, lambda e: e.tensor_tensor(out, in0, in1, op), reads, writes)

    def cp(eng, out, in_, reads, writes):
        if eng == "act":
            return sc.op("act", lambda e: e.copy(out, in_), reads, writes)
        return sc.op(eng, lambda e: e.tensor_copy(out, in_), reads, writes)

    def red(out, in_, op, reads, writes):
        return sc.op("dve", lambda e: e.tensor_reduce(out, in_, AX.X, op), reads, writes)

    def recip(out, in_, reads, writes):
        return sc.op("dve", lambda e: e.reciprocal(out, in_), reads, writes)

    def memset(ap, val, writes):
        return sc.op("dve", lambda e: e.memset(ap, val), (), writes)

    wslot = [0]
    WXA = [at("WXA%d" % i, [128, 8, 256], BF16, E0 + 8 * KB + i * 4 * KB) for i in range(4)]
    WXF = [at("WXF%d" % i, [128, 8, 256], BF16, C0 + 44 * KB + i * 4 * KB) for i in range(4)]
    SLOTS = {0: WB[:, 0, :, :], 1: WB[:, 1, :, :]}
    for i in range(4):
        SLOTS[2 + i] = WXA[i][:, :, :]
        SLOTS[6 + i] = WXF[i][:, :, :]
    active = [[0, 1]]

    def set_slots(lst):
        active[0] = list(lst)

    def WS(s):
        return SLOTS[s]

    def wbuf(s):
        return B("WB%d" % s)

    def next_slot():
        s = active[0][wslot[0] % len(active[0])]
        wslot[0] += 1
        return s

    def w3(name, c0, ncols, k0=0, nk=8):
        return dr[name].rearrange("(k p) c -> p k c", p=128)[:, k0:k0 + nk, c0:c0 + ncols]

    def load_w(name, c0, ncols, k0=0, nk=8, slot=None, kdst=0, cdst=0):
        s = next_slot() if slot is None else slot
        dma("pool", WS(s)[:, kdst:kdst + nk, cdst:cdst + ncols], w3(name, c0, ncols, k0, nk), writes=[wbuf(s)])
        return s

    for nm, t in [("ident", ident), ("bones", bones), ("omean", omean), ("ones", ones), ("qaug", qaug),
                  ("kaug", kaug), ("corr", corr), ("negblk", negblk), ("c_fm", cfm), ("ndist", ndist), ("qaug", qaugd), ("bk", BK),
                  ("b_ada_fm", bfm), ("n1g", n1g), ("n2g", n2g), ("qkg", qkg), ("subg", subg),
                  ("lamv", lamv)]:
        dma("sp", t[:, :], dr[nm], writes=[CONST])
    memset(EPSC[:, :], EPS, [CONST])
    memset(onesf[:, :], 1.0, [CONST])
    memset(lsc[:, :], 0.0, [CONST])
    sc.barrier()

    if stop_after == "consts":
        nb = 0
    act(csT[:, :], cfm[:, :], AF.Silu, [], [B("csT")])
    ts("dve", qkg[:, 0:1], qkg[:, 0:1], 0.125, None, ALU.mult, None, [], [B("qkg")])
    ts("dve", qkg[:, 2:3], qkg[:, 2:3], 0.125, None, ALU.mult, None, [B("qkg")], [B("qkg")])
    ts("dve", subg[:, :], subg[:, :], 0.8, None, ALU.mult, None, [], [B("subg")])
    tt("dve", lamv[0:1, 0:64], lamv[0:1, 0:64], lamv[0:1, 64:128], ALU.mult, [], [B("lamv")])
    tt("dve", lamv[0:1, 128:192], lamv[0:1, 128:192], lamv[0:1, 192:256], ALU.mult, [B("lamv")], [B("lamv")])
    red(lsc[0:1, 0:1], lamv[0:1, 0:64], ALU.add, [B("lamv")], [B("lsc")])
    red(lsc[0:1, 1:2], lamv[0:1, 128:192], ALU.add, [B("lamv")], [B("lsc")])
    act(lsc[0:1, 2:4], lsc[0:1, 0:2], AF.Exp, [B("lsc")], [B("lsc2")])
    tt("dve", lsc[0:1, 4:5], lsc[0:1, 3:4], lsc[0:1, 2:3], ALU.subtract, [B("lsc2")], [B("lsc3")])
    ts("dve", lsc[0:1, 5:6], lsc[0:1, 4:5], -0.2, None, ALU.add, None, [B("lsc3")], [B("lsc4")])
    mm(PS[7][:, 0:2], onesf[0:1, :], lsc[0:1, 5:7], True, True, [B("lsc4")], [psb[7]])
    cp("dve", nlam[:, :], PS[7][:, 0:1], [psb[7]], [B("nlam")])

    MP = PS[6]
    for g in [0, 1, 2, 3, 6, 7, 8, 9]:
        for hf in range(2):
            s = load_w("w_ada", g * 512 + hf * 256, 256)
            for jj in range(2):
                j = g * 4 + hf * 2 + jj
                for k in range(8):
                    mm(MP[:, j:j + 49:48], WS(s)[:, k, jj * 128:(jj + 1) * 128], csT[:, 2 * k:2 * k + 2],
                       k == 0, k == 7, [wbuf(s), B("csT")], [psb[6]])
    for b in range(2):
        for (c0_, c1_) in [(0, 16), (24, 40)]:
            tt("dve", modfm[:, b * 48 + c0_:b * 48 + c1_], MP[:, b * 48 + c0_:b * 48 + c1_], bfm[:, c0_:c1_], ALU.add,
               [psb[6]], [B("modfm")])
        stt(A1[:, b * 8:(b + 1) * 8], modfm[:, b * 48 + 8:b * 48 + 16], 1.0, n1g[:, 0:8], ALU.add, ALU.mult,
            [B("modfm")], [B("A1")])
        stt(A2[:, b * 8:(b + 1) * 8], modfm[:, b * 48 + 32:b * 48 + 40], 1.0, n2g[:, 0:8], ALU.add, ALU.mult,
            [B("modfm")], [B("A2")])
    sc.barrier()

    def gate_bc(b, gi):
        g0 = [2048, 5120][gi]
        dma("sp", GBC[:, :], dr["bg"][:, gi * 1024:(gi + 1) * 1024], writes=[B("GBC")])
        for k in range(8):
            ts("dve", CSR[:, k, :], ones[:, :], csT[:, 2 * k + b:2 * k + b + 1], None, ALU.mult, None, [], [B("CSR")])
        for n in range(2):
            ss = [load_w("w_ada", g0 + n * 512 + hf * 256, 256) for hf in range(2)]
            for hf in range(2):
                for k in range(8):
                    mm(PS[7][:, hf * 256:(hf + 1) * 256], CSR[:, k, :], WS(ss[hf])[:, k, 0:256], k == 0, k == 7,
                       [B("CSR"), wbuf(ss[hf])], [psb[7]])
            tt("dve", GBC[:, n * 512:(n + 1) * 512], PS[7][:, :], GBC[:, n * 512:(n + 1) * 512],
               ALU.add, [psb[7], B("GBC")], [B("GBC")])
        sc.barrier()

    def layer_norm(b, from_x1, Acol, Bcol0, dstT, dstname, junk):
        for i in range(16):
            sl_ = i % 2
            if from_x1:
                xin = x1[:, i, :]
                xb = B("x1_%d" % (i // 4))
            else:
                xin = XT[:, sl_, :]
                xb = B("XT%d" % sl_)
                dma("sp", XT[:, sl_, :], dr["x"][b, i * 128:(i + 1) * 128, :], writes=[xb])
            act(junk, xin, AF.Square, [xb], [B("junk"), B("ss")], accum_out=ST[:, 0:1])
            act(ST[:, 1:2], ST[:, 0:1], AF.Sqrt, [B("ss")], [B("sd")], bias=EPSC[:, 0:1], scale=1.0 / D)
            recip(ST[:, 2:3], ST[:, 1:2], [B("sd")], [B("rstd")])
            ts("dve", XN[:, sl_, :], xin, ST[:, 2:3], None, ALU.mult, None, [xb, B("rstd")], [B("XN%d" % sl_)])
            pst = PSB[6 + sl_]
            for k in range(8):
                tr(pst[:, k * 128:(k + 1) * 128], XN[:, sl_, k * 128:(k + 1) * 128], [B("XN%d" % sl_)],
                   [psb[6 + sl_]])
            for k in range(8):
                act(dstT[:, k, i * 128:(i + 1) * 128], pst[:, k * 128:(k + 1) * 128], AF.Identity,
                    [psb[6 + sl_]], [B("%s_%d" % (dstname, i // 4))],
                    scale=Acol[:, b * 8 + k:b * 8 + k + 1],
                    bias=modfm[:, b * 48 + Bcol0 + k:b * 48 + Bcol0 + k + 1])

    def proj_fm(c0, norm_gcol, dst, dstbuf, dup64=False):
        if dup64:
            s = next_slot()
            load_w("w_in", c0, 64, slot=s)
            load_w("w_in", c0, 64, slot=s, cdst=64)
        else:
            s = load_w("w_in", c0, 128)
        for n in range(4):
            pn = n % 2
            P1 = PS[pn]
            for k in range(8):
                mm(P1[:, :], WS(s)[:, k, 0:128], hT[:, k, n * 512:(n + 1) * 512], k == 0, k == 7,
                   [wbuf(s), B("hT_%d" % n)], [psb[pn]])
            ob = B("%s_%d" % (dstbuf, n))
            if norm_gcol is None:
                cp("act", dst[:, n * 512:(n + 1) * 512], P1[:, :], [psb[pn]], [ob])
                continue
            act(SQ[:, pn, :], P1[:, :], AF.Square, [psb[pn]], [B("SQ%d" % pn)])
            P2 = PS[2 + pn]
            mm(P2[:, :], bones[:, :], SQ[:, pn, :], True, True, [B("SQ%d" % pn)], [psb[2 + pn]])
            act(RS[:, pn, :], P2[:, :], AF.Ln, [psb[2 + pn]], [B("RS%d" % pn)], bias=EPSC[:, 0:1])
            act(RS[:, pn, :], RS[:, pn, :], AF.Exp, [B("RS%d" % pn)], [B("RS%d" % pn)], scale=-0.5)
            stt(dst[:, n * 512:(n + 1) * 512], P1[:, :], qkg[:, norm_gcol:norm_gcol + 1], RS[:, pn, :],
                ALU.mult, ALU.mult, [psb[pn], B("RS%d" % pn)], [ob])

    def proj_v(c0):
        for hf in range(2):
            s = load_w("w_in", c0 + hf * 256, 256)
            for i in range(16):
                pn = i % 2
                for k in range(8):
                    mm(PS[pn][:, 0:256], hT[:, k, i * 128:(i + 1) * 128], WS(s)[:, k, 0:256], k == 0, k == 7,
                       [wbuf(s), B("hT_%d" % (i // 4))], [psb[pn]])
                cp("act" if i % 2 else "dve", V[:, i, hf * 256:(hf + 1) * 256], PS[pn][:, 0:256], [psb[pn]], [B("V")])

    ptc = [0]
    sc.op("pool", lambda e: e.memset(QZ[:, :, :], 0.0), (), [B("QZ0"), B("QZ1")])

    def attn_runs(runs, hook=None):
        blocks = []
        for r, R in enumerate(runs):
            nst = 4 * R["n"] + 4
            for j in range(nst):
                blocks.append((r, j, nst))

        def prep(r):
            if r >= len(runs):
                return
            R = runs[r]
            zs = R["prow"] // 64
            pr = R["prow"]
            cp("pool", QZ[pr:pr + 64, zs, :], QK[pr:pr + 64, R["qt"], R["n"] * 512:(R["n"] + 1) * 512],
               [B("QK%d_%d" % (R["qt"], R["n"]))], [B("QZ%d" % zs)])

        state = {}

        def stage_a(i):
            r, j, nst = blocks[i]
            R = runs[r]
            n, hh, kt, use_mask = R["n"], R["hh"], R["kt"], R["use_mask"]
            zs = R["prow"] // 64
            qa_ = qaugd if use_mask else qaug
            qab = [B("qaugd")] if use_mask else []
            jd = j - 4 * n
            c_lo = 0 if jd < 0 else 128 * jd
            sb_ = ptc[0] % 2
            pslot = ptc[0] % 3
            ptc[0] += 1
            state[i] = (sb_, pslot, c_lo)
            SP_ = PS[sb_]
            ksl = QK[:, kt, j * 128:(j + 1) * 128]
            kb_, qb_ = B("QK%d_%d" % (kt, j // 4)), B("QZ%d" % zs)
            if jd < 0:
                groups = [(0, 512, False)]
            else:
                groups = [(c_lo, c_lo + 128, True)]
                if c_lo + 128 < 512:
                    groups.append((c_lo + 128, 512, False))
            for (a_, bnd, isdiag) in groups:
                if use_mask:
                    mm(SP_[:, a_:bnd], ksl, QZ[:, zs, a_:bnd], True, False, [kb_, qb_], [psb[sb_]])
                    mm(SP_[:, a_:bnd], kaug[:, hh * 128:(hh + 1) * 128], qa_[:, a_:bnd], False, False, qab, [psb[sb_]])
                else:
                    mm(SP_[:, a_:bnd], ksl, QZ[:, zs, a_:bnd], True, not isdiag, [kb_, qb_], [psb[sb_]])
                if use_mask:
                    mm(SP_[:, a_:bnd], ident[:, :], MBT[:, j, a_:bnd], False, not isdiag, [B("MBT")], [psb[sb_]])
                if isdiag:
                    mm(SP_[:, a_:bnd], ident[:, :], corr[:, hh * 128:(hh + 1) * 128], False, True, [], [psb[sb_]])
            if j == nst - 1:
                prep(r + 2)

        def stage_bc(i):
            r, j, nst = blocks[i]
            R = runs[r]
            n, hh = R["n"], R["hh"]
            sb_, pslot, c_lo = state.pop(i)
            if R["use_mask"]:
                imm = -SLOPES[hh] * (512 * n - 128 * j)
                act(PT[:, pslot, c_lo:512], PS[sb_][:, c_lo:512], AF.Exp, [psb[sb_]], [B("PT%d" % pslot)],
                    bias=IMM[:, immcol(imm):immcol(imm) + 1])
            else:
                for gg in range(c_lo // 256, 2):
                    lo_ = max(c_lo, 256 * gg)
                    dl = j - 4 * n - 2 * gg
                    col = (hh - 8) * 17 + dl + 15
                    act(PT[:, pslot, lo_:256 * gg + 256], PS[sb_][:, lo_:256 * gg + 256], AF.Exp, [psb[sb_]],
                        [B("PT%d" % pslot)], bias=BK[:, col:col + 1])
            ob_, sbk = R["obank"], R["sbank"]
            vc = R["vcols"]
            mm(PS[ob_][:, c_lo:512], V[:, j, vc:vc + 128], PT[:, pslot, c_lo:512], j == 0, j == nst - 1,
               [B("V"), B("PT%d" % pslot)], [psb[ob_]])
            mm(PS[sbk][:, c_lo:512], ones[:, :], PT[:, pslot, c_lo:512], j == 0, j == nst - 1,
               [B("PT%d" % pslot)], [psb[sbk]])
            if j == nst - 1:
                R["fin"]()

        prep(0)
        prep(1)
        stage_a(0)
        for i in range(len(blocks)):
            if i + 1 < len(blocks):
                stage_a(i + 1)
            stage_bc(i)
            if hook is not None:
                hook()

    def act_recip(dst, src, rb, wb):
        act(dst, src, AF.Ln, rb, wb)
        act(dst, dst, AF.Exp, wb, wb, scale=-1.0)

    immvals = sorted({-sp_ * (512 * n - 128 * j) for sp_ in set(SLOPES) for n in range(4) for j in range(4 * n + 4)})
    immidx = {v: i for i, v in enumerate(immvals)}
    IMM = salloc("IMM", [128, len(immvals)], F32)
    assert base[0] <= 229344, base[0]

    def immcol(v):
        return immidx[v]

    for v, i in immidx.items():
        memset(IMM[:, i:i + 1], float(v), [CONST])
    sc.barrier()

    def locals_():
        return dict(hT=hT, QK=QK, V=V, QI=QI, KI=KI, WI=WI, yT=yT, x1=x1, h2T=h2T, mrgT=mrgT, MBT=MBT,
                    IDX=IDX, modfm=modfm, GBC=GBC, BIS=BIS, A1=A1, nlam=nlam, MB=MB)

    if stop_after == "p0":
        nb = 0
    for b in range(nb):
        layer_norm(b, False, A1, 0, hT, "hT", JUNKA[:, 0:D])
        sc.barrier()
        if stop_after == "ln1":
            break

        set_slots([0, 1, 2, 3, 4, 5])
        for p in range(4):
            proj_fm(C_QA + p * 128, 0, QK[:, p, :], "QK%d" % p)
            proj_fm(C_KA + p * 128, 1, QK[:, 4 + p, :], "QK%d" % (4 + p))
        proj_v(C_VA)
        for p in range(2):
            proj_fm(C_QI + p * 128, None, QI[:, p, :], "QI")
        proj_fm(C_KI, None, KI[:, :], "KI", dup64=True)
        s = load_w("w_in", C_WI, 4)
        for i in range(16):
            for k in range(8):
                mm(PS[7][:, i * 4:(i + 1) * 4], hT[:, k, i * 128:(i + 1) * 128], WS(s)[:, k, 0:4], k == 0, k == 7,
                   [wbuf(s), B("hT_%d" % (i // 4))], [psb[7]])
        cp("dve", WI[:, 0:64], PS[7][:, 0:64], [psb[7]], [B("WI")])
        sc.barrier()
        set_slots([0, 1])
        if stop_after == "proj_a":
            break

        dma("sp", TIEF[:, :], dr["tief"], writes=[B("TIEF")])
        memset(DMINX[:, :, :], 0.0, [B("DMINX")])
        MBs = [MB[:, 0, :], MB[:, 1, :], WB[:, 0, :, :].rearrange("p k c -> p (k c)"),
               WB[:, 1, :, :].rearrange("p k c -> p (k c)")]

        def maskA(n):
            for half in range(2):
                tiles = [4 * n + 2 * half, 4 * n + 2 * half + 1]
                for gi, i in enumerate(tiles):
                    N = (i + 1) * 128
                    idxb = B("IDX%d" % gi)
                    for jb in range((N + 511) // 512):
                        cols = min(512, N - jb * 512)
                        for h in range(4):
                            pr = (h % 2) * 64
                            pb = 2 + (h % 2)
                            mm(PS[pb][:, 0:cols], QI[pr:pr + 64, h // 2, i * 128:(i + 1) * 128],
                               KI[pr:pr + 64, jb * 512:jb * 512 + cols], True, True, [B("QI"), B("KI")], [psb[pb]])
                            rs_ = h % 2
                            act(T1[:, rs_, 0:cols], PS[pb][:, 0:cols], AF.Relu, [psb[pb]], [B("T1%d" % rs_)])
                            dst = IDX[:, gi, jb * 512:jb * 512 + cols]
                            if h == 0:
                                ts("dve", dst, T1[:, rs_, 0:cols], WI[:, i * 4:i * 4 + 1], None, ALU.mult, None,
                                   [B("T1%d" % rs_), B("WI")], [idxb])
                            else:
                                stt(dst, T1[:, rs_, 0:cols], WI[:, i * 4 + h:i * 4 + h + 1], dst, ALU.mult, ALU.add,
                                    [B("T1%d" % rs_), B("WI"), idxb], [idxb])
                        yield
                    tt("dve", IDX[:, gi, i * 128:(i + 1) * 128], IDX[:, gi, i * 128:(i + 1) * 128], negblk[:, :],
                       ALU.add, [idxb], [idxb])
                    if i >= 2:
                        stt(IDX[:, gi, 0:N], IDX[:, gi, 0:N], 0.0, IDX[:, gi, 0:N], ALU.is_ge, ALU.add, [idxb], [idxb])
                        ts("dve", JUNK8[:, 0:N], IDX[:, gi, 0:N], 1.0, None, ALU.is_equal, None, [idxb], [B("junk")])
                        sc.op("dve", lambda e, gi=gi, N=N: e.copy_predicated(IDX[:, gi, 0:N], JUNK8[:, 0:N], TIEF[:, 0:N]),
                              [B("junk"), B("TIEF")], [idxb])
                    yield
                bis = B("BIS")
                if tiles[0] >= 2:
                    for gi, i in enumerate(tiles):
                        N = (i + 1) * 128
                        red(BIS[:, gi:gi + 1], IDX[:, gi, 0:N], ALU.max, [B("IDX%d" % gi)], [bis])
                        red(BIS[:, 2 + gi:3 + gi], IDX[:, gi, 0:N - 64], ALU.min, [B("IDX%d" % gi)], [bis])
                    stt(BIS[:, 4:6], BIS[:, 0:2], 1.0, BIS[:, 2:4], ALU.add, ALU.subtract, [bis], [bis])
                    yield
                    for it in range(NBIS):
                        stt(BIS[:, 6:8], BIS[:, 4:6], 2.0 ** (-(it + 1)), BIS[:, 2:4], ALU.mult, ALU.add, [bis], [B("MID")])
                        N0 = (tiles[0] + 1) * 128
                        N1 = (tiles[1] + 1) * 128
                        ts("dve", JUNK8[:, 0:N0], IDX[:, 0, 0:N0], BIS[:, 6:7], 0.0, ALU.is_ge, ALU.add,
                           [B("IDX0"), B("MID")], [B("junk"), B("CNT0")], accum_out=BIS[:, 8:9])
                        act(JUNK2[:, 0:N1], IDX[:, 1, 0:N1], AF.Sign, [B("IDX1"), B("MID")], [B("junk2"), B("CNT1")],
                            bias=BIS[:, 7:8], scale=-1.0, accum_out=BIS[:, 9:10])
                        ts("dve", BISU[:, 0:1], BIS[:, 8:9], float(TOPK), None, ALU.is_ge, None,
                           [B("CNT0")], [B("BD")])
                        ts("dve", BISU[:, 1:2], BIS[:, 9:10], float(N1) - 511.5, None, ALU.is_le, None,
                           [B("CNT1")], [B("BD")])
                        sc.op("dve", lambda e: e.copy_predicated(BIS[:, 2:4], BISU[:, 0:2], BIS[:, 6:8]),
                              [B("BD"), B("MID")], [bis])
                        yield
                else:
                    memset(BIS[:, 2:4], -1e29, [bis])
                for gi, i in enumerate(tiles):
                    N = (i + 1) * 128
                    g4 = i - 4 * n
                    mbt_ = MBs[g4]
                    ts("dve", mbt_[:, 0:N], IDX[:, gi, 0:N], BIS[:, 2 + gi:3 + gi], NEG, ALU.is_lt, ALU.mult,
                       [B("IDX%d" % gi), bis], [B("MB%d" % g4)])
                    tt("dve", IDX[:, gi, 0:N], mbt_[:, 0:N], ndist[:, 1920 - 128 * i:1920 - 128 * i + N], ALU.add,
                       [B("MB%d" % g4), B("IDX%d" % gi)], [B("IDX%d" % gi)])
                    red(DMIN[:, g4:g4 + 1], IDX[:, gi, 0:N], ALU.max, [B("IDX%d" % gi)], [B("DMIN")])
                    cp("dve", DMINX[:, g4, g4:g4 + 1], DMIN[:, g4:g4 + 1], [B("DMIN")], [B("DMINX")])
                    yield

        def maskB(n):
            for g4 in range(4):
                i = 4 * n + g4
                for j0 in range(0, i + 1, 4):
                    js = list(range(j0, min(j0 + 4, i + 1)))
                    pb = 4 + ((j0 // 4) % 2)
                    for jj, j in enumerate(js):
                        tr(PSB[pb][:, jj * 128:(jj + 1) * 128], MBs[g4][:, j * 128:(j + 1) * 128],
                           [B("MB%d" % g4)], [psb[pb]])
                    for jj, j in enumerate(js):
                        cp("act" if jj % 2 else "dve", MBT[:, j, g4 * 128:(g4 + 1) * 128],
                           PSB[pb][:, jj * 128:(jj + 1) * 128], [psb[pb]], [B("MBT")])
            for g4 in range(4):
                tr(PSB[4][0:4, g4 * 128:(g4 + 1) * 128], DMINX[:, g4, 0:4], [B("DMINX")], [psb[4]])
            cp("dve", qaugd[0:4, 0:512], PSB[4][0:4, 0:512], [psb[4]], [B("qaugd")])

        for _ in maskA(0):
            pass
        maskB(0)
        for n in range(4):
            gen = maskA(n + 1) if n < 3 else iter(())
            nblk = 8 * (4 * n + 4)
            nyield = 2 * (NBIS + 12) if n < 3 else 0
            stepacc = [0.0]

            def hook(gen=gen, per=nyield / float(nblk)):
                stepacc[0] += per
                while stepacc[0] >= 1.0:
                    stepacc[0] -= 1.0
                    next(gen, None)

            runs = []
            for h in range(8):
                ob, sb2 = (4, 5) if h % 2 == 0 else (6, 7)
                pr = (h % 2) * 64

                def fin(h=h, ob=ob, sb2=sb2, pr=pr, n=n):
                    rsl = h % 2
                    act_recip(RS[pr:pr + 64, rsl, :], PS[sb2][pr:pr + 64, :], [psb[sb2]], [B("RS%d" % rsl)])
                    tt("dve", yT[pr:pr + 64, h // 2, n * 512:(n + 1) * 512], PS[ob][pr:pr + 64, :], RS[pr:pr + 64, rsl, :],
                       ALU.mult, [psb[ob], B("RS%d" % rsl)], [B("yT_%d" % n)])
                runs.append(dict(n=n, hh=h, qt=h // 2, kt=4 + h // 2, prow=pr, vcols=(h // 2) * 128, obank=ob, sbank=sb2,
                                 use_mask=True, fin=fin))
            attn_runs(runs, hook)
            for _ in gen:
                pass
            if n < 3:
                maskB(n + 1)
        sc.barrier()
        if stop_after in ("dsa", "mask"):
            break

        set_slots([0, 1, 2, 3, 4, 5])
        for p in range(4):
            proj_fm(C_QB + p * 128, 2, QK[:, p, :], "QK%d" % p)
            proj_fm(C_KB + p * 128, 3, QK[:, 4 + p, :], "QK%d" % (4 + p))
        proj_v(C_VB)
        set_slots([0, 1])
        for n in range(4):
            runs = []
            for h in range(4):
                def fin1():
                    recip(T1[:, 0, :], PS[5][:, :], [psb[5]], [B("T10")])
                    tt("dve", RS[:, 0, :], PS[4][:, :], T1[:, 0, :], ALU.mult, [psb[4], B("T10")], [B("RS0")])

                def fin2(h=h, n=n):
                    recip(T1[:, 1, :], PS[7][:, :], [psb[7]], [B("T11")])
                    tt("dve", RS[:, 1, :], PS[6][:, :], T1[:, 1, :], ALU.mult, [psb[6], B("T11")], [B("RS1")])
                    stt(RS[:, 0, :], RS[:, 1, :], nlam[:, 0:1], RS[:, 0, :], ALU.mult, ALU.add,
                        [B("RS0"), B("RS1")], [B("RS0")])
                    act(SQ[:, 0, :], RS[:, 0, :], AF.Square, [B("RS0")], [B("SQ0")])
                    mm(PS[2][:, :], omean[:, :], SQ[:, 0, :], True, True, [B("SQ0")], [psb[2]])
                    act(T1[:, 0, :], PS[2][:, :], AF.Ln, [psb[2]], [B("T10")], bias=EPSC[:, 0:1])
                    act(T1[:, 0, :], T1[:, 0, :], AF.Exp, [B("T10")], [B("T10")], scale=-0.5)
                    stt(yT[:, 4 + h, n * 512:(n + 1) * 512], RS[:, 0, :], subg[:, 0:1], T1[:, 0, :], ALU.mult, ALU.mult,
                        [B("RS0"), B("T10")], [B("yT_%d" % n)])
                runs.append(dict(n=n, hh=8 + h, qt=h, kt=4 + h, prow=0, vcols=h * 128, obank=4, sbank=5,
                                 use_mask=False, fin=fin1))
                runs.append(dict(n=n, hh=8 + h, qt=h, kt=4 + h, prow=64, vcols=h * 128, obank=6, sbank=7,
                                 use_mask=False, fin=fin2))
            attn_runs(runs)
        sc.barrier()
        if stop_after == "diff":
            break

        set_slots([0, 1, 2, 3, 4, 5])
        for c in range(8):
            sga = load_w("w_in", C_G + c * 128, 128)
            sgb = load_w("w_in", C_G + D + c * 128, 128)
            for n in range(4):
                for gsel, sg in enumerate([sga, sgb]):
                    for k in range(8):
                        mm(PS[gsel][:, :], WS(sg)[:, k, 0:128], hT[:, k, n * 512:(n + 1) * 512], k == 0, k == 7,
                           [wbuf(sg), B("hT_%d" % n)], [psb[gsel]])
                    act(GTf[:, gsel, n * 512:(n + 1) * 512], PS[gsel][:, :], AF.Sigmoid, [psb[gsel]],
                        [B("GT%d_%d" % (gsel, n))])
            su = next_slot()
            load_w("w_up_a", c * 128, 128, 0, 4, slot=su, kdst=0)
            load_w("w_up_b", c * 128, 128, 0, 4, slot=su, kdst=4)
            for n in range(4):
                for kk in range(4):
                    mm(PS[2][:, :], WS(su)[:, kk, 0:128], yT[:, kk, n * 512:(n + 1) * 512], kk == 0, kk == 3,
                       [wbuf(su), B("yT_%d" % n)], [psb[2]])
                for kk in range(4):
                    mm(PS[3][:, :], WS(su)[:, 4 + kk, 0:128], yT[:, 4 + kk, n * 512:(n + 1) * 512], kk == 0, kk == 3,
                       [wbuf(su), B("yT_%d" % n)], [psb[3]])
                tt("dve", T1[:, 0, :], PS[2][:, :], GTf[:, 0, n * 512:(n + 1) * 512], ALU.mult,
                   [psb[2], B("GT0_%d" % n)], [B("T10")])
                tt("dve", T1[:, 1, :], PS[3][:, :], GTf[:, 1, n * 512:(n + 1) * 512], ALU.mult,
                   [psb[3], B("GT1_%d" % n)], [B("T11")])
                tt("pool", mrgT[:, c, n * 512:(n + 1) * 512], T1[:, 0, :], T1[:, 1, :], ALU.add,
                   [B("T10"), B("T11")], [B("mrgT_%d" % n)])
        sc.barrier()
        set_slots([0, 1])

        gate_bc(b, 0)
        for q4 in range(4):
            dma("pool", Wo[:, :, q4 * 256:(q4 + 1) * 256], w3("w_o", q4 * 256, 256), writes=[B("Wo")])
        for i in range(16):
            dma("sp", x1[:, i, :], dr["x"][b, i * 128:(i + 1) * 128, :], writes=[B("x1_%d" % (i // 4))])
        for i in range(16):
            for dd in range(2):
                for c in range(8):
                    mm(PS[dd][:, :], mrgT[:, c, i * 128:(i + 1) * 128], Wo[:, c, dd * 512:(dd + 1) * 512], c == 0, c == 7,
                       [B("mrgT_%d" % (i // 4)), B("Wo")], [psb[dd]])
                tt("dve", T1[:, dd, :], PS[dd][:, :], GBC[:, dd * 512:(dd + 1) * 512], ALU.mult,
                   [psb[dd]], [B("T1%d" % dd)])
                tt("pool", x1[:, i, dd * 512:(dd + 1) * 512], x1[:, i, dd * 512:(dd + 1) * 512], T1[:, dd, :], ALU.add,
                   [B("T1%d" % dd), B("x1_%d" % (i // 4))], [B("x1_%d" % (i // 4))])
        sc.barrier()
        if stop_after == "x1":
            break

        layer_norm(b, True, A2, 24, h2T, "h2T", JUNKE[:, 0:D])
        sc.barrier()

        gate_bc(b, 1)
        for th in range(2):
            set_slots([0, 1, 6, 7, 8, 9])
            for fp in range(11):
                sa = load_w("w_ff1", fp * 256, 256)
                sb = load_w("w_ff3", fp * 256, 256)
                for ff in range(2):
                    f = 2 * fp + ff
                    for nn in range(2):
                        n = 2 * th + nn
                        p0 = 2 * ((2 * ff + nn) % 2)
                        for k in range(8):
                            mm(PS[p0][:, :], WS(sa)[:, k, ff * 128:(ff + 1) * 128], h2T[:, k, n * 512:(n + 1) * 512],
                               k == 0, k == 7, [wbuf(sa), B("h2T_%d" % n)], [psb[p0]])
                        for k in range(8):
                            mm(PS[p0 + 1][:, :], WS(sb)[:, k, ff * 128:(ff + 1) * 128], h2T[:, k, n * 512:(n + 1) * 512],
                               k == 0, k == 7, [wbuf(sb), B("h2T_%d" % n)], [psb[p0 + 1]])
                        sq = (2 * ff + nn) % 2
                        act(SQ[:, sq, :], PS[p0][:, :], AF.Silu, [psb[p0]], [B("SQ%d" % sq)])
                        tt("dve", uT[:, f, nn * 512:(nn + 1) * 512], PS[p0 + 1][:, :], SQ[:, sq, :], ALU.mult,
                           [psb[p0 + 1], B("SQ%d" % sq)], [B("uT")])
            sc.barrier()
            set_slots([0, 1])
            def load_w2(e):
                dma("pool", w2bufs[e % 2][:, :, :],
                    dr["w_ff2"].rearrange("(f p) d -> p f d", p=128)[:, :, e * 128:(e + 1) * 128],
                    writes=[B("W2_%d" % (e % 2))])

            load_w2(0)
            for e8 in range(8):
                if e8 + 1 < 8:
                    load_w2(e8 + 1)
                wt = w2bufs[e8 % 2]
                for g in range(8):
                    pg = 4 + (g % 2)
                    i = 8 * th + g
                    for f in range(22):
                        mm(PS[pg][:, 0:128], uT[:, f, g * 128:(g + 1) * 128], wt[:, f, :], f == 0, f == 21,
                           [B("uT"), B("W2_%d" % (e8 % 2))], [psb[pg]])
                    tt("dve", T1[:, g % 2, 0:128], PS[pg][:, 0:128], GBC[:, e8 * 128:(e8 + 1) * 128], ALU.mult,
                       [psb[pg]], [B("T1%d" % (g % 2))])
                    tt("pool", x1[:, i, e8 * 128:(e8 + 1) * 128], x1[:, i, e8 * 128:(e8 + 1) * 128], T1[:, g % 2, 0:128],
                       ALU.add, [B("T1%d" % (g % 2)), B("x1o_%d" % i)], [B("x1o_%d" % i)])
            for g in range(8):
                i = 8 * th + g
                dma("sp", out_d[b, i * 128:(i + 1) * 128, :], x1[:, i, :], reads=[B("x1o_%d" % i)], writes=[B("outd")])
            sc.barrier()

    loc = locals_()
    for name, v in dbg.items():
        dma("sp", dbg_d[name], v[0](loc), writes=[B("dbgout")])
    sc.barrier()

    sems = {e: nc.alloc_semaphore("s_" + e) for e in ("pe", "act", "dve", "pool")}
    dsems = {"sp": [nc.alloc_semaphore("dsp%d" % i) for i in range(8)],
             "pool": [nc.alloc_semaphore("dpl%d" % i) for i in range(6)]}
    sc.prepare(sems, dsems)
    stats = {}
    with nc.Block() as block:

        @block.tensor
        def _(e):
            stats["pe"] = sc.emit_engine("pe", e)

        @block.scalar
        def _(e):
            stats["act"] = sc.emit_engine("act", e)

        @block.vector
        def _(e):
            stats["dve"] = sc.emit_engine("dve", e)

        @block.gpsimd
        def _(e):
            stats["pool"] = sc.emit_engine("pool", e)

        @block.sync
        def _(e):
            stats["sp"] = sc.emit_engine("sp", e)

    return nc, stats


def _core_inputs(inp, core):
    f = np.float32
    g = lambda k: np.asarray(inp[k], dtype=f)
    b0 = core * NB
    c = g("c")[b0:b0 + NB]
    c_fm = np.ascontiguousarray(c.reshape(NB, 8, 128).transpose(2, 1, 0).reshape(128, 16))
    b_ada = g("b_ada")[0]
    m = dict(
        x=np.ascontiguousarray(g("x")[b0:b0 + NB]),
        c_fm=c_fm,
        w_ada=np.ascontiguousarray(g("w_ada")[0]),
        b_ada_fm=np.ascontiguousarray(b_ada.reshape(48, 128).T),
        bg=np.ascontiguousarray(np.broadcast_to(np.concatenate([b_ada[2048:3072], b_ada[5120:6144]])[None, :], (128, 2048))),
        n1g=np.ascontiguousarray(g("norm1_g")[0].reshape(8, 128).T),
        n2g=np.ascontiguousarray(g("norm2_g")[0].reshape(8, 128).T),
        w_in=np.ascontiguousarray(g("w_in")[0]),
        qkg=np.ascontiguousarray(np.stack([np.tile(g("qn_a")[0], 2), np.tile(g("kn_a")[0], 2),
                                           np.tile(g("qn_b")[0], 2), np.tile(g("kn_b")[0], 2)], axis=1)),
        subg=np.ascontiguousarray(g("subln_g")[0].reshape(128, 1)),
        lamv=np.ascontiguousarray(np.concatenate([g("lam_q1")[0], g("lam_k1")[0], g("lam_q2")[0], g("lam_k2")[0]])[None, :]),
        w_up_a=np.ascontiguousarray(g("w_up_a")[0]), w_up_b=np.ascontiguousarray(g("w_up_b")[0]),
        w_o=np.ascontiguousarray(g("w_o")[0]), w_ff1=np.ascontiguousarray(g("w_ff1")[0]),
        w_ff3=np.ascontiguousarray(g("w_ff3")[0]), w_ff2=np.ascontiguousarray(g("w_ff2")[0]),
    )
    return m


_CACHE = {}


def kernel(**inputs):
    consts = _consts()
    shared = None
    in_maps = []
    for core in range(NCORES):
        m = _core_inputs(inputs, core)
        if shared is None:
            shared = {k: m[k] for k in m if k not in ("x", "c_fm")}
        else:
            for k in shared:
                m[k] = shared[k]
        m.update(consts)
        in_maps.append(m)
    if "nc" not in _CACHE:
        _CACHE["nc"] = build()[0]
    nc = _CACHE["nc"]
    res = run_bass_kernel_spmd(nc, in_maps, core_ids=list(range(NCORES)))
    outs = [np.asarray(r["out"], dtype=np.float32) for r in res.results]
    return np.concatenate(outs, axis=0)
```
